# Optimizing a Trainium2 kernel written in Bass

```python
import jax, jax.numpy as jnp
from jax import lax
import numpy as np

D_MODEL = 2048
BATCH = 4
SEQ = 4096
DEPTH = 4

CHUNK = 64
HG_HEADS = 8
HG_KDIM = 128
HG_VDIM = 128
HG_FDIM = HG_HEADS * HG_KDIM
HG_WIDTH = HG_HEADS * HG_VDIM
F_MIN = 1e-6
SA_HEADS = 16
SA_KV_LATENT = 256
SA_Q_RANK = 512
SA_V_DIM = 64
SA_WIDTH = SA_HEADS * SA_V_DIM
IDX_HEADS = 16
IDX_DIM = 64
TOPK_MAX = 256
Q_BLOCK = 128
MASK_VALUE = -1e30
D_FF = ((8 * D_MODEL + 3 * 256 - 1) // (3 * 256)) * 256
ALPHA = (2 * DEPTH) ** 0.25
BETA = (8 * DEPTH) ** -0.25
ADA_SCALE = 0.1
IN_SPLITS = (HG_FDIM, HG_FDIM, HG_WIDTH, HG_WIDTH, SA_Q_RANK, SA_KV_LATENT, IDX_DIM, IDX_HEADS, D_MODEL, D_MODEL)
D_IN = sum(IN_SPLITS)
HG_I_START = 2 * HG_FDIM

kernel_name = 'hybrid_hgrn2_dsa_deepnorm_encoder'


def _split_points(sizes):
    pts, acc = [], 0
    for s in sizes[:-1]:
        acc += s
        pts.append(acc)
    return pts


def layer_norm(x, g, b, eps=1e-5):
    xf = x.astype(jnp.float32)
    mu = jnp.mean(xf, -1, keepdims=True)
    var = jnp.mean(jnp.square(xf - mu), -1, keepdims=True)
    return ((xf - mu) * lax.rsqrt(var + eps)).astype(x.dtype) * g + b


def rms_norm(x, g, eps=1e-6):
    xf = x.astype(jnp.float32)
    return (xf * lax.rsqrt(jnp.mean(xf * xf, -1, keepdims=True) + eps)).astype(x.dtype) * g


def alibi_slopes(n):
    return jnp.exp2(-8.0 * (jnp.arange(n, dtype=jnp.float32) + 1.0) / n)


def hgrn_lower_bounds(lb_logits):
    p = jax.nn.softmax(lb_logits.astype(jnp.float32), axis=0)
    return jnp.cumsum(p, axis=0) - p[0:1]


def hgrn2_chunkwise(q, k, v, log_f):
    B, L, H, K = q.shape
    V = v.shape[-1]
    nc = L // CHUNK

    def to_chunks(t):
        return t.astype(jnp.float32).reshape(B, nc, CHUNK, H, t.shape[-1]).transpose(1, 0, 3, 2, 4)

    causal = jnp.tril(jnp.ones((CHUNK, CHUNK), bool))[:, :, None]

    def step(S, inp):
        qt, kt, vt, ft = inp
        b = jnp.cumsum(ft, axis=2)
        b_last = b[:, :, -1:, :]
        o_inter = jnp.einsum('bhtk,bhkv->bhtv', qt * jnp.exp(b), S)
        diff = b[:, :, :, None, :] - b[:, :, None, :, :]
        decay = jnp.where(causal, jnp.exp(jnp.where(causal, diff, 0.0)), 0.0)
        scores = jnp.einsum('bhtk,bhtsk,bhsk->bhts', qt, decay, kt)
        o_intra = jnp.einsum('bhts,bhsv->bhtv', scores, vt)
        S_new = jnp.exp(b_last[:, :, 0, :])[..., None] * S + jnp.einsum('bhsk,bhsv->bhkv', kt * jnp.exp(b_last - b), vt)
        return S_new, o_inter + o_intra

    S0 = jnp.zeros((B, H, K, V), jnp.float32)
    _, o = lax.scan(step, S0, (to_chunks(q), to_chunks(k), to_chunks(v), to_chunks(log_f)))
    return o.transpose(1, 0, 3, 2, 4).reshape(B, L, H, V)


def dsa_attention(q, c_kv, q_idx, k_idx, idx_w, slopes):
    B, L, H, C = q.shape
    n_sel = min(TOPK_MAX, L // 4)
    nblk = L // Q_BLOCK
    pos = jnp.arange(L)
    key_chunk = pos // CHUNK
    scale = C ** -0.5

    def blocks(t):
        return jnp.moveaxis(t.reshape(B, nblk, Q_BLOCK, *t.shape[2:]), 1, 0)

    def one_block(inp):
        qb, qib, wb, pb = inp
        q_chunk = pb // CHUNK
        isc = jax.nn.relu(jnp.einsum('bqhd,bsd->bqhs', qib, k_idx))
        isc = jnp.einsum('bqhs,bqh->bqs', isc, wb).astype(jnp.float32)
        admissible = key_chunk[None, :] <= q_chunk[:, None]
        isc = jnp.where(admissible[None], isc, MASK_VALUE)
        _, sel = lax.top_k(isc, n_sel)
        kv = jax.vmap(lambda t, i: t[i])(c_kv, sel)
        valid = (sel // CHUNK) <= q_chunk[None, :, None]
        dist = jnp.abs(pb[None, :, None] - sel).astype(jnp.float32)
        s = jnp.einsum('bqhc,bqkc->bqhk', qb, kv).astype(jnp.float32) * scale
        s = s - slopes[None, None, :, None] * dist[:, :, None, :]
        s = jnp.where(valid[:, :, None, :], s, MASK_VALUE)
        p = jax.nn.softmax(s, axis=-1).astype(kv.dtype)
        return jnp.einsum('bqhk,bqkc->bqhc', p, kv)

    o = lax.map(one_block, (blocks(q), blocks(q_idx), blocks(idx_w), pos.reshape(nblk, Q_BLOCK)))
    return jnp.moveaxis(o, 0, 1).reshape(B, L, H, C)


def mixer_sublayer(h, lb, slopes, w_in, g_hg, w_pa, g_cq, g_ckv, w_uq, w_iq, w_uv, w_pb, w_out):
    B, L, _ = h.shape
    proj = h @ w_in
    q_a, f_a, i_a, g_a, c_q, c_kv, k_idx, idx_w, gate_a, gate_b = jnp.split(proj, _split_points(IN_SPLITS), axis=-1)

    heads = lambda t: t.reshape(B, L, HG_HEADS, -1)
    lb_h = lb.reshape(HG_HEADS, HG_KDIM)
    f = lb_h + (1.0 - lb_h) * jax.nn.sigmoid(heads(f_a).astype(jnp.float32))
    log_f = jnp.log(jnp.clip(f, F_MIN, 1.0))
    k_a = 1.0 - f
    o_a = hgrn2_chunkwise(heads(q_a), k_a, heads(i_a), log_f).astype(h.dtype)
    y_a = (rms_norm(o_a, g_hg.reshape(HG_HEADS, HG_VDIM)) * jax.nn.silu(heads(g_a))).reshape(B, L, HG_WIDTH)

    c_q = rms_norm(c_q, g_cq)
    q_b = (c_q @ w_uq).reshape(B, L, SA_HEADS, SA_KV_LATENT)
    c_kv = rms_norm(c_kv, g_ckv)
    q_idx = (c_q @ w_iq).reshape(B, L, IDX_HEADS, IDX_DIM)
    idx_w = idx_w * (IDX_HEADS ** -0.5 * IDX_DIM ** -0.5)
    o_b = dsa_attention(q_b, c_kv, q_idx, k_idx, idx_w, slopes)
    y_b = jnp.einsum('blhc,hcv->blhv', o_b, w_uv).reshape(B, L, SA_WIDTH)

    merged = jax.nn.sigmoid(gate_a) * (y_a @ w_pa) + jax.nn.sigmoid(gate_b) * (y_b @ w_pb)
    return merged @ w_out


def swiglu(h, w_gate, w_up, w_down):
    return (jax.nn.silu(h @ w_gate) * (h @ w_up)) @ w_down


def setup_inputs(seed: int = 0) -> dict:
    key = jax.random.key(seed)
    ks = jax.random.split(key, 22)
    nrm = lambda k, shape, s: jax.random.normal(k, shape, jnp.float32) * s
    gain = lambda k, shape: 1.0 + 0.02 * jax.random.normal(k, shape, jnp.float32)
    col_scale = jnp.ones((D_IN,), jnp.float32).at[HG_I_START:HG_I_START + HG_WIDTH].set(BETA)
    return {
        'x': nrm(ks[0], (BATCH, SEQ, D_MODEL), 1.0),
        'c': nrm(ks[1], (BATCH, D_MODEL), 1.0),
        'w_ada': nrm(ks[2], (DEPTH, D_MODEL, 6 * D_MODEL), ADA_SCALE * D_MODEL ** -0.5),
        'b_ada': nrm(ks[3], (DEPTH, 6 * D_MODEL), 0.01),
        'w_in': nrm(ks[4], (DEPTH, D_MODEL, D_IN), D_MODEL ** -0.5) * col_scale,
        'lb_logits': nrm(ks[5], (DEPTH, HG_FDIM), 0.5),
        'g_hg': gain(ks[6], (DEPTH, HG_WIDTH)),
        'w_pa': nrm(ks[7], (DEPTH, HG_WIDTH, D_MODEL), HG_WIDTH ** -0.5),
        'g_cq': gain(ks[8], (DEPTH, SA_Q_RANK)),
        'g_ckv': gain(ks[9], (DEPTH, SA_KV_LATENT)),
        'w_uq': nrm(ks[10], (DEPTH, SA_Q_RANK, SA_HEADS * SA_KV_LATENT), SA_Q_RANK ** -0.5),
        'w_iq': nrm(ks[11], (DEPTH, SA_Q_RANK, IDX_HEADS * IDX_DIM), SA_Q_RANK ** -0.5),
        'w_uv': nrm(ks[12], (DEPTH, SA_HEADS, SA_KV_LATENT, SA_V_DIM), BETA * SA_KV_LATENT ** -0.5),
        'w_pb': nrm(ks[13], (DEPTH, SA_WIDTH, D_MODEL), SA_WIDTH ** -0.5),
        'w_out': nrm(ks[14], (DEPTH, D_MODEL, D_MODEL), BETA * D_MODEL ** -0.5),
        'ln1_g': gain(ks[15], (DEPTH, D_MODEL)),
        'ln1_b': nrm(ks[16], (DEPTH, D_MODEL), 0.01),
        'w_gate': nrm(ks[17], (DEPTH, D_MODEL, D_FF), D_MODEL ** -0.5),
        'w_up': nrm(ks[18], (DEPTH, D_MODEL, D_FF), BETA * D_MODEL ** -0.5),
        'w_down': nrm(ks[19], (DEPTH, D_FF, D_MODEL), BETA * D_FF ** -0.5),
        'ln2_g': gain(ks[20], (DEPTH, D_MODEL)),
        'ln2_b': nrm(ks[21], (DEPTH, D_MODEL), 0.01),
    }


def reference(x, c, w_ada, b_ada, w_in, lb_logits, g_hg, w_pa, g_cq, g_ckv, w_uq, w_iq, w_uv, w_pb, w_out, ln1_g, ln1_b, w_gate, w_up, w_down, ln2_g, ln2_b):
    lbs = hgrn_lower_bounds(lb_logits)
    slopes = alibi_slopes(SA_HEADS)
    cond = jax.nn.silu(c)
    for l in range(DEPTH):
        mod = (cond @ w_ada[l] + b_ada[l])[:, None, :]
        sh_m, sc_m, gt_m, sh_f, sc_f, gt_f = jnp.split(mod, 6, axis=-1)
        h = x * (1.0 + sc_m) + sh_m
        y = mixer_sublayer(h, lbs[l], slopes, w_in[l], g_hg[l], w_pa[l], g_cq[l], g_ckv[l], w_uq[l], w_iq[l], w_uv[l], w_pb[l], w_out[l])
        x = layer_norm(ALPHA * x + (1.0 + gt_m) * y, ln1_g[l], ln1_b[l])
        h = x * (1.0 + sc_f) + sh_f
        y = swiglu(h, w_gate[l], w_up[l], w_down[l])
        x = layer_norm(ALPHA * x + (1.0 + gt_f) * y, ln2_g[l], ln2_b[l])
    return x
```

```python
import numpy as np
from contextlib import ExitStack
import concourse.bass as bass
import concourse.mybir as mybir
from concourse.bass_utils import run_bass_kernel_spmd

F32, BF16 = mybir.dt.float32, mybir.dt.bfloat16
AF = mybir.ActivationFunctionType
ALU = mybir.AluOpType
AX = mybir.AxisListType

D = 2048
KC = 16
HG = 8
DFF = 5632
FC = 44
NH = 16
G = 256
NQB = G // 128
CH = G // 64
ALPHA_ = 8.0 ** 0.25
SLOPES = [float(2.0 ** (-8.0 * (i + 1) / 16)) for i in range(16)]
BIG = 1.0e30


class Sched:
    def __init__(self, nc, es):
        self.nc, self.es = nc, es
        self.eng = {'pe': nc.tensor, 'act': nc.scalar, 'dve': nc.vector, 'pool': nc.gpsimd, 'sp': nc.sync}
        self.sem, self.cnt, self.nsem = {}, {}, 0
        for e in self.eng:
            self._newsem(e)
        self.lastw, self.readers = {}, {}
        self.waited = {e: {} for e in self.eng}
        self.dsem = {}

    def _newsem(self, e):
        s = self.es.enter_context(self.nc.semaphore(f"s{self.nsem}_{e}"))
        self.nsem += 1
        self.sem[e] = s
        self.cnt[e] = 0

    def _wait(self, e, tk, raw):
        sem, val, src = tk
        if src == e and (e in ('pe', 'sp', 'pool') or not raw):
            return
        w = self.waited[e]
        if w.get(id(sem), 0) >= val:
            return
        self.eng[e].wait_ge(sem, val)
        w[id(sem)] = val

    def _deps(self, e, reads, writes):
        for r in reads:
            t = self.lastw.get(r)
            if t:
                self._wait(e, t, True)
        for w_ in writes:
            t = self.lastw.get(w_)
            if t:
                self._wait(e, t, False)
            for t in self.readers.get(w_, {}).values():
                self._wait(e, t, False)

    def _commit(self, tk, reads, writes):
        for w_ in writes:
            self.lastw[w_] = tk
            self.readers[w_] = {}
        for r in reads:
            d = self.readers.setdefault(r, {})
            d[id(tk[0])] = tk

    def op(self, e, fn, reads=(), writes=()):
        self._deps(e, reads, writes)
        ins = fn()
        if self.cnt[e] >= 30000:
            self._newsem(e)
        self.cnt[e] += 1
        ins.then_inc(self.sem[e], 1)
        tk = (self.sem[e], self.cnt[e], e)
        self._commit(tk, reads, writes)
        return tk

    def dma(self, q, out, in_, reads, writes, res):
        if res not in self.dsem:
            self.dsem[res] = [self.es.enter_context(self.nc.semaphore(f"d{len(self.dsem)}")), 0]
        ds = self.dsem[res]
        if ds[1] >= 30000 * 16:
            ds[0] = self.es.enter_context(self.nc.semaphore(f"d{len(self.dsem)}_{self.nsem}"))
            self.nsem += 1
            ds[1] = 0
        self._deps(q, reads, writes)
        ins = self.eng[q].dma_start(out=out, in_=in_)
        ds[1] += 16
        ins.then_inc(ds[0], 16)
        tk = (ds[0], ds[1], 'dma')
        self._commit(tk, reads, writes)
        return tk

    def barrier(self, engs=('pe', 'act', 'dve')):
        for e in engs:
            for o in engs:
                if o != e and self.cnt[o] > 0:
                    self._wait(e, (self.sem[o], self.cnt[o], o), True)

    def finish(self):
        for res, (sem, cnt) in self.dsem.items():
            if cnt:
                self._wait('sp', (sem, cnt, 'dma'), True)
        for o in self.eng:
            if o != 'sp' and self.cnt[o] > 0:
                self._wait('sp', (self.sem[o], self.cnt[o], o), True)


def build(L, DEPTH):
    NG = L // G
    NSB = L // 128
    NSEL = min(256, L // 4)
    NROUND = NSEL // 8
    nc = bass.Bass("TRN2", target_bir_lowering=False)
    es = ExitStack()

    def din(name, shape):
        return nc.dram_tensor(name, list(shape), F32, kind="ExternalInput").ap()

    xin = din("xin", [128, KC, L])
    xout = nc.dram_tensor("xout", [128, KC, L], F32, kind="ExternalOutput").ap()
    xs = [nc.dram_tensor(f"xs{i}", [128, KC, L], F32, kind="Internal").ap() for i in range(2)]
    cvec = din("cvec", [128, KC])
    w_ada = din("w_ada", [DEPTH, 96, 128, KC * 128])
    b_ada = din("b_ada", [128, DEPTH, 96])
    w_infm = din("w_infm", [DEPTH, 31, 128, KC * 128])
    w_ini = din("w_ini", [DEPTH, 4, 128, KC * 256])
    w_inw = din("w_inw", [DEPTH, 128, KC * 16])
    w_ing = din("w_ing", [DEPTH, 32, 128, KC * 128])
    lbl = din("lbl", [128, DEPTH, HG])
    g_hg = din("g_hg", [128, DEPTH, HG])
    g_cq = din("g_cq", [128, DEPTH, 4])
    g_ckv = din("g_ckv", [128, DEPTH, 2])
    w_iq = din("w_iq", [DEPTH, 8, 128, 4 * 128])
    w_uq = din("w_uq", [DEPTH, 32, 128, 4 * 128])
    w_uv = din("w_uv", [DEPTH, 8, 128, 4 * 128])
    w_pa = din("w_pa", [DEPTH, 16, 128, 8 * 128])
    w_pb = din("w_pb", [DEPTH, 16, 128, 8 * 128])
    w_out = din("w_out", [DEPTH, 16, 128, KC * 128])
    w_gate = din("w_gate", [DEPTH, FC, 128, KC * 128])
    w_up = din("w_up", [DEPTH, FC, 128, KC * 128])
    w_down = din("w_down", [DEPTH, 32, 128, 22 * 128])
    lnp = din("lnp", [128, DEPTH, 4, KC])
    c_ident = din("c_ident", [128, 128])
    c_tri = din("c_tri", [64, 64])
    c_reset = din("c_reset", [128, G])

    def sb(name, shape, dt):
        return es.enter_context(nc.sbuf_tensor(name, list(shape), dt))

    S = Sched(nc, es)
    V, A, P, PE = nc.vector, nc.scalar, nc.gpsimd, nc.tensor

    wsrc = dict(w_infm=w_infm, w_ini=w_ini, w_inw=w_inw, w_ing=w_ing, w_iq=w_iq, w_uq=w_uq, w_uv=w_uv, w_pa=w_pa,
                w_pb=w_pb, w_out=w_out, w_gate=w_gate, w_up=w_up, w_down=w_down)
    wbf = {}
    for name, a in wsrc.items():
        wbf[name] = nc.dram_tensor(name + "_bf", list(a.shape), BF16, kind="Internal").ap()
    w_infm, w_ini, w_inw, w_ing, w_iq, w_uq, w_uv, w_pa, w_pb, w_out, w_gate, w_up, w_down = [
        wbf[k] for k in ("w_infm", "w_ini", "w_inw", "w_ing", "w_iq", "w_uq", "w_uv", "w_pa", "w_pb", "w_out", "w_gate", "w_up", "w_down")]
    cur_layer = [0]

    xT = sb("xT", [128, KC, G], F32)
    hT = sb("hT", [128, KC, G], BF16)
    R1 = sb("R1", [128, 2 * L * 4], mybir.dt.uint8) if False else None
    r1_bytes = max(2 * L * 4, FC * G * 2, 28 * 1024)
    R1 = sb("R1", [128, r1_bytes // 4], F32)
    R2 = sb("R2", [128, max(NSB * G * 2, 16 * 1024) // 4], F32)

    def view(reg, off_bytes, shape, dt):
        esz = 2 if dt == BF16 else 4
        n = int(np.prod(shape[1:]))
        a = reg[0:shape[0], off_bytes // 4:(off_bytes + n * esz) // 4]
        if dt == BF16:
            a = a.bitcast(BF16)
        if len(shape) == 3:
            a = a.rearrange("p (a b) -> p a b", b=shape[2])
        elif len(shape) == 4:
            a = a.rearrange("p (a b c) -> p a b c", b=shape[2], c=shape[3])
        return a

    o = 0
    ke = view(R1, o, [128, HG, G], BF16); o += HG * G * 2
    qe = view(R1, o, [128, HG, G], BF16); o += HG * G * 2
    sgT = view(R1, o, [128, HG, G], BF16); o += HG * G * 2
    vtm = view(R1, o, [64, CH, 1024], BF16); o += CH * 1024 * 2
    hgt = []
    for i in range(7):
        hgt.append(view(R1, o, [128, G], F32)); o += G * 4
    hg_f, hg_kk, hg_lf, hg_b, hg_br, hg_e1, hg_e2 = hgt
    assert o <= r1_bytes, (o, r1_bytes)
    isc = view(R1, 0, [128, L], F32)
    work = view(R1, L * 4, [128, L], F32)
    hmid = view(R1, 0, [128, FC, G], BF16)
    ndT = view(R2, 0, [128, NSB, G], BF16)
    merged = view(R2, 0, [128, KC, G], BF16)
    o = KC * G * 2
    tmpA = view(R2, o, [128, G], F32); o += G * 4
    tmpB = view(R2, o, [128, G], F32); o += G * 4
    tmpC = view(R2, o, [128, G], F32); o += G * 4

    yaT = sb("yaT", [128, HG, G], BF16)
    ybT = sb("ybT", [128, 8, G], BF16)
    Sst = sb("Sst", [128, HG, 128], F32)
    Sr = sb("Sr", [128, 2, 128], BF16)
    scm = sb("scm", [64, 2, 64], BF16)
    keT = sb("keT", [64, 2, CH * 128], BF16)
    tmpSU = sb("tmpSU", [128, 2, 128], F32)
    hg_sc = sb("hg_sc", [128, HG, 3, CH], F32)
    cq = sb("cq", [128, 4, G], F32)
    ckvr = sb("ckvr", [128, 2, G], F32)
    cqn = sb("cqn", [128, 4, G], BF16)
    sqb = [sb(f"sqb{i}", [128, G], BF16) for i in range(2)]
    vbb = [sb(f"vbb{i}", [128, G], BF16) for i in range(2)]
    st_sd = sb("st_sd", [128, G], F32)
    st_rs = sb("st_rs", [128, G], F32)
    st_mean = sb("st_mean", [128, G], F32)
    st_var = sb("st_var", [128, G], F32)
    oTs = sb("oTs", [128, G], F32)
    qidxT = sb("qidxT", [128, 8, G], BF16)
    idxw = sb("idxw", [128, NQB, NH], F32)
    diag = sb("diag", [128, NQB, NH, 128], BF16)
    selm1 = sb("selm1", [128, L], mybir.dt.int8)
    Rb = [sb(f"Rb{i}", [128, 512], BF16) for i in range(3)]
    m8 = sb("m8", [128, 8], F32)
    thr = sb("thr", [128, 1], F32)
    ndmin = sb("ndmin", [128, 1], F32)
    qbT = sb("qbT", [128, 2, 2, G], BF16)
    tbuf = [sb(f"tbuf{i}", [128, G], F32) for i in range(4)]
    PTb = [sb(f"PT{i}", [128, G], BF16) for i in range(4)]
    obT = sb("obT", [128, 2, 2, G], BF16)
    rden = sb("rden", [128, G], F32)
    ckvT_all = sb("ckvT_all", [128, 2, L], BF16)
    ckv_tm = sb("ckv_tm", [128, NSB, 256], BF16)
    kidxT_all = sb("kidxT_all", [128, L], BF16)
    NW = 3
    wst = [sb(f"wst{i}", [128, 22 * 128], BF16) for i in range(NW)]
    wtm = sb("wtm", [128, KC * 256], BF16)
    wtmw = sb("wtmw", [128, KC * 16], BF16)
    ident_bf = sb("ident_bf", [128, 128], BF16)
    ident_f = sb("ident_f", [128, 128], F32)
    ones_bf = sb("ones_bf", [128, 128], BF16)
    tri = sb("tri", [64, 64], F32)
    resetm = sb("resetm", [128, G], F32)
    modt = sb("modt", [128, DEPTH, 96], F32)
    badat = sb("badat", [128, DEPTH, 96], F32)
    lnpt = sb("lnpt", [128, DEPTH, 4, KC], F32)
    lbt = sb("lbt", [128, DEPTH, HG], F32)
    omlt = sb("omlt", [128, DEPTH, HG], F32)
    lbtmp = sb("lbtmp", [128, 3, HG], F32)
    ghgt = sb("ghgt", [128, DEPTH, HG], F32)
    gcqt = sb("gcqt", [128, DEPTH, 4], F32)
    gckvt = sb("gckvt", [128, DEPTH, 2], F32)
    ct = sb("ct", [128, KC], F32)
    condb = sb("condb", [128, KC], BF16)

    ps = [es.enter_context(nc.psum_tensor(f"ps{i}", [128, 512], F32)) for i in range(8)]
    PS = [f"ps{i}" for i in range(8)]
    psT = ps[7][:].bitcast(BF16)

    cst = [view(R2, i * 4096, [128, 2048], BF16) for i in range(2)]
    ci = 0
    for l in range(DEPTH):
        for name, a in wsrc.items():
            s3 = a[l] if len(a.shape) == 4 else a[l:l + 1]
            d3 = wbf[name][l] if len(a.shape) == 4 else wbf[name][l:l + 1]
            T_, _, cols = s3.shape
            csz = cols if cols <= 2048 else cols // 2
            for t_ in range(T_):
                for c0 in range(0, cols, csz):
                    k = ci % 2
                    ci += 1
                    S.dma('pool', cst[k][:, 0:csz], s3[t_, :, c0:c0 + csz], (), (f"cst{k}",), f"cst{k}")
                    S.dma('sp', d3[t_, :, c0:c0 + csz], cst[k][:, 0:csz], (f"cst{k}",), (f"wcast{l}",), f"wcast{l}")
    wslot = [0]

    def load_w(src, ncols, cast=False):
        i = wslot[0] % NW
        wslot[0] += 1
        if cast:
            S.dma('pool', wst[i][:, 0:ncols], src, (), (f"wst{i}",), f"wst{i}")
        else:
            q = 'sp' if (wslot[0] % 2) else 'pool'
            S.dma(q, wst[i][:, 0:ncols], src, (f"wcast{cur_layer[0]}",), (f"wst{i}",), f"wst{i}")
        return wst[i], f"wst{i}"

    pctr = [0]

    def proj(src, nkc, rhs_fn, rhs_res, banks=(0, 1), n=G):
        wt, wres = load_w(src, nkc * 128)
        b = banks[pctr[0] % len(banks)]
        pctr[0] += 1
        for kc in range(nkc):
            S.op('pe', lambda kc=kc: PE.matmul(ps[b][:, 0:n], wt[:, kc * 128:(kc + 1) * 128], rhs_fn(kc),
                                               start=(kc == 0), stop=(kc == nkc - 1)),
                 reads=(wres,) + tuple(rhs_res), writes=(PS[b],))
        return ps[b][:, 0:n], PS[b]

    def act(out, in_, func, reads, writes, scale=1.0, bias=0.0):
        return S.op('act', lambda: A.activation(out=out, in_=in_, func=func, scale=scale, bias=bias), reads, writes)

    def ts(out, in0, s1, s2, op0, op1, reads, writes):
        if op1 is None:
            return S.op('dve', lambda: V.tensor_scalar(out=out, in0=in0, scalar1=s1, scalar2=None, op0=op0), reads, writes)
        return S.op('dve', lambda: V.tensor_scalar(out=out, in0=in0, scalar1=s1, scalar2=s2, op0=op0, op1=op1), reads, writes)

    def tt(out, in0, in1, op, reads, writes):
        return S.op('dve', lambda: V.tensor_tensor(out=out, in0=in0, in1=in1, op=op), reads, writes)

    def stt(out, in0, sc, in1, op0, op1, reads, writes):
        return S.op('dve', lambda: V.scalar_tensor_tensor(out=out, in0=in0, scalar=sc, in1=in1, op0=op0, op1=op1), reads, writes)

    def rstd_from_sum(psum_ap, psres, inv_n, eps):
        ts(st_var[:], psum_ap, inv_n, eps, ALU.mult, ALU.add, (psres,), ("st_var",))
        act(st_sd[:], st_var[:], AF.Sqrt, ("st_var",), ("st_sd",))
        S.op('dve', lambda: V.reciprocal(out=st_rs[:], in_=st_sd[:]), ("st_sd",), ("st_rs",))

    S.dma('pool', ident_bf[:], c_ident, (), ("ident_bf",), "c0")
    S.dma('sp', ident_f[:], c_ident, (), ("ident_f",), "c1")
    S.dma('sp', tri[:], c_tri, (), ("tri",), "c2")
    S.dma('sp', resetm[:], c_reset, (), ("resetm",), "c3")
    S.dma('sp', badat[:], b_ada, (), ("badat",), "c4")
    S.dma('sp', lnpt[:], lnp, (), ("lnpt",), "c5")
    S.dma('sp', lbt[:], lbl, (), ("lbt",), "c6")
    S.dma('sp', ghgt[:], g_hg, (), ("ghgt",), "c7")
    S.dma('sp', gcqt[:], g_cq, (), ("gcqt",), "c8")
    S.dma('sp', gckvt[:], g_ckv, (), ("gckvt",), "c9")
    S.dma('sp', ct[:], cvec, (), ("ct",), "c10")
    S.op('dve', lambda: V.memset(ones_bf[:], 1.0), (), ("ones_bf",))
    S.op('dve', lambda: V.memset(Sst[:], 0.0), (), tuple(f"Sst{h}" for h in range(HG)))
    act(condb[:], ct[:], AF.Silu, ("ct",), ("condb",))
    for l in range(DEPTH):
        for mc in range(96):
            wt, wres = load_w(w_ada[l, mc], KC * 128, cast=True)
            for kc in range(KC):
                S.op('pe', lambda kc=kc: PE.matmul(ps[0][:, mc:mc + 1], wt[:, kc * 128:(kc + 1) * 128],
                                                   condb[:, kc:kc + 1], start=(kc == 0), stop=(kc == KC - 1)),
                     (wres, "condb"), (PS[0],))
        tt(modt[:, l, :], ps[0][:, 0:96], badat[:, l, :], ALU.add, (PS[0], "badat"), ("modt",))
    for l in range(DEPTH):
        for c0 in (16, 32, 64, 80):
            ts(modt[:, l, c0:c0 + 16], modt[:, l, c0:c0 + 16], 1.0, None, ALU.add, None, ("modt",), ("modt",))
    mx, sm, rc = lbtmp[:, 0, :], lbtmp[:, 1, :], lbtmp[:, 2, :]
    S.op('dve', lambda: V.tensor_copy(out=mx, in_=lbt[:, 0, :]), ("lbt",), ("lbtmp",))
    for l in range(1, DEPTH):
        tt(mx, mx, lbt[:, l, :], ALU.max, ("lbt", "lbtmp"), ("lbtmp",))
    for l in range(DEPTH):
        tt(lbt[:, l, :], lbt[:, l, :], mx, ALU.subtract, ("lbt", "lbtmp"), ("lbt",))
    act(lbt[:], lbt[:], AF.Exp, ("lbt",), ("lbt",))
    S.op('dve', lambda: V.tensor_copy(out=sm, in_=lbt[:, 0, :]), ("lbt",), ("lbtmp",))
    for l in range(1, DEPTH):
        tt(sm, sm, lbt[:, l, :], ALU.add, ("lbt", "lbtmp"), ("lbtmp",))
    S.op('dve', lambda: V.reciprocal(out=rc, in_=sm), ("lbtmp",), ("lbtmp",))
    for l in range(DEPTH):
        tt(lbt[:, l, :], lbt[:, l, :], rc, ALU.mult, ("lbt", "lbtmp"), ("lbt",))
    S.op('dve', lambda: V.memset(lbt[:, 0, :], 0.0), ("lbt",), ("lbt",))
    for l in range(2, DEPTH):
        tt(lbt[:, l, :], lbt[:, l, :], lbt[:, l - 1, :], ALU.add, ("lbt",), ("lbt",))
    ts(omlt[:], lbt[:], -1.0, 1.0, ALU.mult, ALU.add, ("lbt",), ("omlt",))

    def layer_norm(l, gi, bi):
        for mc in range(KC):
            act(vbb[mc % 2][:], xT[:, mc, :], AF.Copy, ("xT",), (f"vbb{mc % 2}",))
            act(sqb[mc % 2][:], xT[:, mc, :], AF.Square, ("xT",), (f"sqb{mc % 2}",))
            S.op('pe', lambda: PE.matmul(ps[4][:, 0:G], ones_bf[:], vbb[mc % 2][:], start=(mc == 0), stop=(mc == KC - 1)),
                 ("ones_bf", f"vbb{mc % 2}"), (PS[4],))
            S.op('pe', lambda: PE.matmul(ps[5][:, 0:G], ones_bf[:], sqb[mc % 2][:], start=(mc == 0), stop=(mc == KC - 1)),
                 ("ones_bf", f"sqb{mc % 2}"), (PS[5],))
        ts(st_mean[:], ps[4][:, 0:G], 1.0 / D, None, ALU.mult, None, (PS[4],), ("st_mean",))
        tt(st_sd[:], st_mean[:], st_mean[:], ALU.mult, ("st_mean",), ("st_sd",))
        stt(st_var[:], ps[5][:, 0:G], 1.0 / D, st_sd[:], ALU.mult, ALU.subtract, (PS[5], "st_sd"), ("st_var",))
        ts(st_var[:], st_var[:], 1e-5, None, ALU.add, None, ("st_var",), ("st_var",))
        act(st_sd[:], st_var[:], AF.Sqrt, ("st_var",), ("st_sd",))
        S.op('dve', lambda: V.reciprocal(out=st_rs[:], in_=st_sd[:]), ("st_sd",), ("st_rs",))
        for mc in range(KC):
            tt(xT[:, mc, :], xT[:, mc, :], st_mean[:], ALU.subtract, ("xT", "st_mean"), ("xT",))
            tt(xT[:, mc, :], xT[:, mc, :], st_rs[:], ALU.mult, ("xT", "st_rs"), ("xT",))
            act(xT[:, mc, :], xT[:, mc, :], AF.Identity, ("xT", "lnpt"), ("xT",),
                scale=lnpt[:, l, gi, mc:mc + 1], bias=lnpt[:, l, bi, mc:mc + 1])

    def modulate(l, sc_off, sh_off):
        for kc in range(KC):
            act(hT[:, kc, :], xT[:, kc, :], AF.Identity, ("xT", "modt"), ("hT",),
                scale=modt[:, l, sc_off + kc:sc_off + kc + 1], bias=modt[:, l, sh_off + kc:sh_off + kc + 1])
        act(xT[:], xT[:], AF.Copy, ("xT",), ("xT",), scale=ALPHA_)

    hT_rhs = lambda kc: hT[:, kc, :]

    for l in range(DEPTH):
        cur_layer[0] = l
        src = xin if l == 0 else xs[(l - 1) % 2]
        dst = xout if l == DEPTH - 1 else xs[l % 2]
        if l > 0:
            sem, cnt = S.dsem["xst"]
            S._wait('sp', (sem, cnt, 'dma'), True)
            S.op('dve', lambda: V.memset(Sst[:], 0.0), (), tuple(f"Sst{h}" for h in range(HG)))
        for g in range(NG):
            t0 = g * G
            nsb = (t0 + G) // 128
            S.barrier()
            S.dma('sp', xT[:], src[:, :, t0:t0 + G], (), ("xT",), "xT")
            modulate(l, 16, 0)
            for h in range(HG):
                pf, pfr = proj(w_infm[l, h], KC, hT_rhs, ("hT",))
                act(hg_f[:], pf, AF.Sigmoid, (pfr,), ("hg_f",))
                ts(hg_f[:], hg_f[:], omlt[:, l, h:h + 1], lbt[:, l, h:h + 1], ALU.mult, ALU.add, ("hg_f", "omlt", "lbt"), ("hg_f",))
                ts(hg_kk[:], hg_f[:], -1.0, 1.0, ALU.mult, ALU.add, ("hg_f",), ("hg_kk",))
                ts(hg_f[:], hg_f[:], 1e-6, 1.0, ALU.max, ALU.min, ("hg_f",), ("hg_f",))
                act(hg_lf[:], hg_f[:], AF.Ln, ("hg_f",), ("hg_lf",))
                S.op('dve', lambda: V.tensor_tensor_scan(out=hg_b[:], data0=resetm[:], data1=hg_lf[:], initial=0.0,
                                                         op0=ALU.mult, op1=ALU.add), ("resetm", "hg_lf"), ("hg_b",))
                b3 = hg_b[:].rearrange("p (c t) -> p c t", t=64)
                br3 = hg_br[:].rearrange("p (c t) -> p c t", t=64)
                tt(br3, b3, b3[:, :, 31:32].to_broadcast([128, CH, 64]), ALU.subtract, ("hg_b",), ("hg_br",))
                act(hg_e1[:], hg_br[:], AF.Exp, ("hg_br",), ("hg_e1",))
                act(hg_e2[:], hg_br[:], AF.Exp, ("hg_br",), ("hg_e2",), scale=-1.0)
                act(hg_sc[:, h, 0, :], b3[:, :, 31], AF.Exp, ("hg_b",), ("hg_sc",))
                act(hg_sc[:, h, 1, :], b3[:, :, 63], AF.Exp, ("hg_b",), ("hg_sc",))
                act(hg_sc[:, h, 2, :], br3[:, :, 63], AF.Exp, ("hg_br",), ("hg_sc",))
                tt(ke[:, h, :], hg_kk[:], hg_e2[:], ALU.mult, ("hg_kk", "hg_e2"), ("ke",))
                pq, pqr = proj(w_infm[l, 8 + h], KC, hT_rhs, ("hT",))
                tt(qe[:, h, :], pq, hg_e1[:], ALU.mult, (pqr, "hg_e1"), ("qe",))
                pg, pgr = proj(w_infm[l, 16 + h], KC, hT_rhs, ("hT",))
                act(sgT[:, h, :], pg, AF.Silu, (pgr,), ("sgT",))
            for mc in range(4):
                p_, pr = proj(w_infm[l, 24 + mc], KC, hT_rhs, ("hT",))
                act(cq[:, mc, :], p_, AF.Copy, (pr,), ("cq",))
            for mc in range(2):
                p_, pr = proj(w_infm[l, 28 + mc], KC, hT_rhs, ("hT",))
                act(ckvr[:, mc, :], p_, AF.Copy, (pr,), ("ckvr",))
            p_, pr = proj(w_infm[l, 30], KC, hT_rhs, ("hT",))
            act(kidxT_all[:, t0:t0 + G], p_, AF.Copy, (pr,), ("kidxT_all",))
            for kc in range(4):
                act(sqb[kc % 2][:], cq[:, kc, :], AF.Square, ("cq",), (f"sqb{kc % 2}",))
                S.op('pe', lambda: PE.matmul(ps[6][:, 0:G], ones_bf[:], sqb[kc % 2][:], start=(kc == 0), stop=(kc == 3)),
                     ("ones_bf", f"sqb{kc % 2}"), (PS[6],))
            rstd_from_sum(ps[6][:, 0:G], PS[6], 1.0 / 512, 1e-6)
            for kc in range(4):
                stt(cqn[:, kc, :], cq[:, kc, :], gcqt[:, l, kc:kc + 1], st_rs[:], ALU.mult, ALU.mult, ("cq", "gcqt", "st_rs"), ("cqn",))
            for kc in range(2):
                act(sqb[kc % 2][:], ckvr[:, kc, :], AF.Square, ("ckvr",), (f"sqb{kc % 2}",))
                S.op('pe', lambda: PE.matmul(ps[6][:, 0:G], ones_bf[:], sqb[kc % 2][:], start=(kc == 0), stop=(kc == 1)),
                     ("ones_bf", f"sqb{kc % 2}"), (PS[6],))
            rstd_from_sum(ps[6][:, 0:G], PS[6], 1.0 / 256, 1e-6)
            for kc in range(2):
                stt(ckvT_all[:, kc, t0:t0 + G], ckvr[:, kc, :], gckvt[:, l, kc:kc + 1], st_rs[:], ALU.mult, ALU.mult,
                    ("ckvr", "gckvt", "st_rs"), ("ckvT_all",))
            for qb in range(NQB):
                for cc in range(2):
                    j = qb * 2 + cc
                    S.op('pe', lambda: PE.transpose(out=psT[:, j * 128:(j + 1) * 128],
                                                    in_=ckvT_all[:, cc, t0 + qb * 128:t0 + (qb + 1) * 128], identity=ident_bf[:]),
                         ("ckvT_all", "ident_bf"), (PS[7],))
            for qb in range(NQB):
                act(ckv_tm[:, t0 // 128 + qb, :], psT[:, qb * 256:(qb + 1) * 256], AF.Copy, (PS[7],), ("ckv_tm",))
            for qtr in range(4):
                S.dma('sp', wtm[:], w_ini[l, qtr], (f"wcast{l}",), ("wtm",), "wtm")
                for c in range(CH):
                    b = 2 + (c % 2)
                    for kc in range(KC):
                        S.op('pe', lambda kc=kc: PE.matmul(ps[b][0:64, 0:256], hT[:, kc, c * 64:(c + 1) * 64],
                                                           wtm[:, kc * 256:(kc + 1) * 256], start=(kc == 0), stop=(kc == KC - 1)),
                             ("hT", "wtm"), (PS[b],))
                    act(vtm[:, c, qtr * 256:(qtr + 1) * 256], ps[b][0:64, 0:256], AF.Copy, (PS[b],), ("vtm",))
            S.dma('sp', wtmw[:], w_inw[l], (f"wcast{l}",), ("wtmw",), "wtmw")
            for qb in range(NQB):
                b = 2 + (qb % 2)
                for kc in range(KC):
                    S.op('pe', lambda kc=kc: PE.matmul(ps[b][:, 0:NH], hT[:, kc, qb * 128:(qb + 1) * 128],
                                                       wtmw[:, kc * 16:(kc + 1) * 16], start=(kc == 0), stop=(kc == KC - 1)),
                         ("hT", "wtmw"), (PS[b],))
                act(idxw[:, qb, :], ps[b][:, 0:NH], AF.Copy, (PS[b],), ("idxw",), scale=1.0 / 32.0)
            for wv in range(4):
                for j in range(2):
                    h = wv * 2 + j
                    half = j * 512
                    for c in range(CH):
                        S.op('pe', lambda: PE.transpose(out=psT[0:64, half + c * 128:half + (c + 1) * 128], in_=ke[:, h, c * 64:(c + 1) * 64],
                                                        identity=ident_bf[:]), ("ke", "ident_bf"), (PS[7],))
                for j in range(2):
                    act(keT[:, j, :], psT[0:64, j * 512:j * 512 + CH * 128], AF.Copy, (PS[7],), (f"keT{j}",))
                for c in range(CH):
                    cs = slice(c * 64, (c + 1) * 64)
                    for j in range(2):
                        h = wv * 2 + j
                        ts(Sr[:, j, :], Sst[:, h, :], hg_sc[:, h, 0, c:c + 1], None, ALU.mult, None, (f"Sst{h}", "hg_sc"), (f"Sr{j}",))
                    for j in range(2):
                        h = wv * 2 + j
                        S.op('pe', lambda: PE.matmul(ps[j][0:64, 0:64], ke[:, h, cs], qe[:, h, cs], start=True, stop=True),
                             ("ke", "qe"), (PS[j],))
                    for j in range(2):
                        tt(scm[:, j, :], ps[j][0:64, 0:64], tri[:], ALU.mult, (PS[j], "tri"), (f"scm{j}",))
                    for j in range(2):
                        h = wv * 2 + j
                        po = ps[2 + j][:, c * 64:(c + 1) * 64]
                        S.op('pe', lambda: PE.matmul(po, Sr[:, j, :], qe[:, h, cs], start=True, stop=False),
                             (f"Sr{j}", "qe"), (PS[2 + j],))
                        S.op('pe', lambda: PE.matmul(po, vtm[:, c, h * 128:(h + 1) * 128], scm[:, j, :], start=False, stop=True),
                             ("vtm", f"scm{j}"), (PS[2 + j],))
                    for j in range(2):
                        h = wv * 2 + j
                        S.op('pe', lambda: PE.matmul(ps[4 + j][:, 0:128], keT[:, j, c * 128:(c + 1) * 128],
                                                     vtm[:, c, h * 128:(h + 1) * 128], start=True, stop=True),
                             (f"keT{j}", "vtm"), (PS[4 + j],))
                    for j in range(2):
                        h = wv * 2 + j
                        ts(tmpSU[:, j, :], ps[4 + j][:, 0:128], hg_sc[:, h, 2, c:c + 1], None, ALU.mult, None,
                           (PS[4 + j], "hg_sc"), (f"tmpSU{j}",))
                        stt(Sst[:, h, :], Sst[:, h, :], hg_sc[:, h, 1, c:c + 1], tmpSU[:, j, :], ALU.mult, ALU.add,
                            (f"Sst{h}", "hg_sc", f"tmpSU{j}"), (f"Sst{h}",))
                for j in range(2):
                    h = wv * 2 + j
                    pO = ps[2 + j][:, 0:G]
                    act(sqb[j][:], pO, AF.Square, (PS[2 + j],), (f"sqb{j}",))
                    act(oTs[:], pO, AF.Copy, (PS[2 + j],), ("oTs",))
                    S.op('pe', lambda: PE.matmul(ps[6][:, 0:G], ones_bf[:], sqb[j][:], start=True, stop=True), ("ones_bf", f"sqb{j}"), (PS[6],))
                    rstd_from_sum(ps[6][:, 0:G], PS[6], 1.0 / 128, 1e-6)
                    stt(oTs[:], oTs[:], ghgt[:, l, h:h + 1], st_rs[:], ALU.mult, ALU.mult, ("oTs", "ghgt", "st_rs"), ("oTs",))
                    tt(yaT[:, h, :], oTs[:], sgT[:, h, :], ALU.mult, ("oTs", "sgT"), ("yaT",))
            for mc in range(8):
                p_, pr = proj(w_iq[l, mc], 4, lambda kc: cqn[:, kc, :], ("cqn",))
                act(qidxT[:, mc, :], p_, AF.Copy, (pr,), ("qidxT",))
            S.barrier()
            for qb in range(NQB):
                for h in range(NH):
                    ts(diag[:, qb, h, :], ident_bf[:], idxw[:, qb, h:h + 1], None, ALU.mult, None, ("ident_bf", "idxw"), ("diag",))

            def indexer(qb):
                Pb = t0 + qb * 128
                n_adm = Pb + 128
                nk5 = (n_adm + 511) // 512
                steps = [(k5, h) for k5 in range(nk5) for h in range(NH)]
                PFI = 2

                def SI(i):
                    k5, h = steps[i]
                    w_ = min(512, n_adm - k5 * 512)
                    b = i % 3
                    pr0 = (h % 2) * 64
                    S.op('pe', lambda: PE.matmul(ps[b][:, 0:w_], qidxT[pr0:pr0 + 64, h // 2, qb * 128:(qb + 1) * 128],
                                                 kidxT_all[pr0:pr0 + 64, k5 * 512:k5 * 512 + w_], start=True, stop=True),
                         ("qidxT", "kidxT_all"), (PS[b],))
                for i in range(min(PFI, len(steps))):
                    SI(i)
                for i, (k5, h) in enumerate(steps):
                    if i + PFI < len(steps):
                        SI(i + PFI)
                    w_ = min(512, n_adm - k5 * 512)
                    b = i % 3
                    rb = Rb[b]
                    ab = 3 + (k5 % 2)
                    act(rb[:, 0:w_], ps[b][:, 0:w_], AF.Relu, (PS[b],), (f"Rb{b}",))
                    S.op('pe', lambda: PE.matmul(ps[ab][:, 0:w_], diag[:, qb, h, :], rb[:, 0:w_], start=(h == 0), stop=(h == NH - 1)),
                         ("diag", f"Rb{b}"), (PS[ab],))
                    if h == NH - 1:
                        act(isc[:, k5 * 512:k5 * 512 + w_], ps[ab][:, 0:w_], AF.Copy, (PS[ab],), ("isc",))
                S.op('dve', lambda: V.memset(isc[0:64, n_adm - 64:n_adm], -BIG), ("isc",), ("isc",))

            indexer(0)
            for qb in range(NQB):
                Pb = t0 + qb * 128
                n_adm = Pb + 128
                S.op('dve', lambda: V.max(out=m8[:], in_=isc[:, 0:n_adm]), ("isc",), ("m8",))
                S.op('dve', lambda: V.match_replace(out=work[:, 0:n_adm], in_to_replace=m8[:], in_values=isc[:, 0:n_adm],
                                                    imm_value=-3.0e38), ("isc", "m8"), ("work",))
                if qb + 1 < NQB:
                    indexer(qb + 1)
                for r in range(1, NROUND):
                    S.op('dve', lambda: V.max(out=m8[:], in_=work[:, 0:n_adm]), ("work",), ("m8",))
                    S.op('dve', lambda: V.match_replace(out=work[:, 0:n_adm], in_to_replace=m8[:], in_values=work[:, 0:n_adm],
                                                        imm_value=-3.0e38), ("work", "m8"), ("work",))
                ts(selm1[:, 0:n_adm], work[:, 0:n_adm], -1.0e38, -1.0, ALU.is_le, ALU.add, ("work",), ("selm1",))
                S.op('pool', lambda: P.iota(work[:, 0:n_adm], pattern=[[1, n_adm]], base=-Pb, channel_multiplier=-1,
                                            allow_small_or_imprecise_dtypes=True), (), ("work",))
                ts(tbuf[0][:, 0:128], work[:, Pb:Pb + 128], -1.0, None, ALU.mult, None, ("work",), ("tbuf0",))
                tt(work[:, Pb:Pb + 128], work[:, Pb:Pb + 128], tbuf[0][:, 0:128], ALU.min, ("work", "tbuf0"), ("work",))
                stt(work[:, 0:n_adm], selm1[:, 0:n_adm], BIG, work[:, 0:n_adm], ALU.mult, ALU.add, ("selm1", "work"), ("work",))
                S.op('dve', lambda: V.memset(work[0:64, n_adm - 64:n_adm], -BIG), ("work",), ("work",))
                S.op('dve', lambda: V.tensor_reduce(out=ndmin[:], in_=work[:, 0:n_adm], axis=AX.X, op=ALU.max), ("work",), ("ndmin",))
                ts(work[:, 0:n_adm], work[:, 0:n_adm], ndmin[:, 0:1], None, ALU.subtract, None, ("work", "ndmin"), ("work",))
                nb = n_adm // 128
                for s0 in range(0, nb, 4):
                    k = min(4, nb - s0)
                    bk = 5 + ((s0 // 4) % 2)
                    for j in range(k):
                        S.op('pe', lambda: PE.transpose(out=ps[bk][:, j * 128:(j + 1) * 128], in_=work[:, (s0 + j) * 128:(s0 + j + 1) * 128],
                                                        identity=ident_f[:]), ("work", "ident_f"), (PS[bk],))
                    act(ndT[:, s0:s0 + k, qb * 128:(qb + 1) * 128], ps[bk][:, 0:k * 128].rearrange("p (a b) -> p a b", b=128),
                        AF.Copy, (PS[bk],), ("ndT",))
                for s_ in range(nb, nsb):
                    S.op('dve', lambda: V.memset(ndT[:, s_, qb * 128:(qb + 1) * 128], -BIG), (), ("ndT",))
            def qb_proj(h_):
                for cc in range(2):
                    p_, pr = proj(w_uq[l, h_ * 2 + cc], 4, lambda kc: cqn[:, kc, :], ("cqn",), banks=(7,))
                    act(qbT[:, h_ % 2, cc, :], p_, AF.Copy, (pr,), (f"qbT{h_ % 2}",), scale=1.0 / 16.0)
            PF = 3
            qb_proj(0)
            for h in range(NH):
                hp = h % 2
                if h + 1 < NH:
                    qb_proj(h + 1)

                def ST(s_):
                    b = s_ % 4
                    for cc in range(2):
                        S.op('pe', lambda: PE.matmul(ps[b][:, 0:G], ckvT_all[:, cc, s_ * 128:(s_ + 1) * 128], qbT[:, hp, cc, :],
                                                     start=(cc == 0), stop=(cc == 1)), ("ckvT_all", f"qbT{hp}"), (PS[b],))
                for s_ in range(min(PF, nsb)):
                    ST(s_)
                for s_ in range(nsb):
                    if s_ + PF < nsb:
                        ST(s_ + PF)
                    b = s_ % 4
                    tb, pt = tbuf[b], PTb[b]
                    stt(tb[:], ndT[:, s_, :], SLOPES[h], ps[b][:, 0:G], ALU.mult, ALU.add, ("ndT", PS[b]), (f"tbuf{b}",))
                    act(pt[:], tb[:], AF.Exp, (f"tbuf{b}",), (f"PT{b}",))
                    S.op('pe', lambda: PE.matmul(ps[4][:, 0:G], ckv_tm[:, s_, 0:128], pt[:], start=(s_ == 0), stop=(s_ == nsb - 1)),
                         ("ckv_tm", f"PT{b}"), (PS[4],))
                    S.op('pe', lambda: PE.matmul(ps[5][:, 0:G], ckv_tm[:, s_, 128:256], pt[:], start=(s_ == 0), stop=(s_ == nsb - 1)),
                         ("ckv_tm", f"PT{b}"), (PS[5],))
                    S.op('pe', lambda: PE.matmul(ps[6][:, 0:G], ones_bf[:], pt[:], start=(s_ == 0), stop=(s_ == nsb - 1)),
                         ("ones_bf", f"PT{b}"), (PS[6],))
                act(obT[:, hp, 0, :], ps[4][:, 0:G], AF.Copy, (PS[4],), ("obT",))
                act(obT[:, hp, 1, :], ps[5][:, 0:G], AF.Copy, (PS[5],), ("obT",))
                S.op('dve', lambda: V.reciprocal(out=rden[hp * 64:hp * 64 + 64, :], in_=ps[6][hp * 64:hp * 64 + 64, 0:G]), (PS[6],), ("rden",))
                if hp == 1:
                    wt, wres = load_w(w_uv[l, h // 2], 4 * 128)
                    for i4 in range(4):
                        S.op('pe', lambda: PE.matmul(ps[7][:, 0:G], wt[:, i4 * 128:(i4 + 1) * 128], obT[:, i4 // 2, i4 % 2, :],
                                                     start=(i4 == 0), stop=(i4 == 3)), (wres, "obT"), (PS[7],))
                    tt(ybT[:, h // 2, :], ps[7][:, 0:G], rden[:], ALU.mult, (PS[7], "rden"), ("ybT",))
            S.barrier()
            for mc in range(KC):
                pa, par = proj(w_pa[l, mc], 8, lambda kc: yaT[:, kc, :], ("yaT",), banks=(0, 1, 2, 3))
                pga, pgar = proj(w_ing[l, mc], KC, hT_rhs, ("hT",), banks=(0, 1, 2, 3))
                act(tmpA[:], pga, AF.Sigmoid, (pgar,), ("tmpA",))
                tt(tmpB[:], pa, tmpA[:], ALU.mult, (par, "tmpA"), ("tmpB",))
                pb_, pbr = proj(w_pb[l, mc], 8, lambda kc: ybT[:, kc, :], ("ybT",), banks=(0, 1, 2, 3))
                pgb, pgbr = proj(w_ing[l, 16 + mc], KC, hT_rhs, ("hT",), banks=(0, 1, 2, 3))
                act(tmpC[:], pgb, AF.Sigmoid, (pgbr,), ("tmpC",))
                tt(tmpC[:], pb_, tmpC[:], ALU.mult, (pbr, "tmpC"), ("tmpC",))
                tt(merged[:, mc, :], tmpB[:], tmpC[:], ALU.add, ("tmpB", "tmpC"), ("merged",))
            for mc in range(KC):
                py, pyr = proj(w_out[l, mc], KC, lambda kc: merged[:, kc, :], ("merged",), banks=(0, 1, 2, 3))
                stt(xT[:, mc, :], py, modt[:, l, 32 + mc:33 + mc], xT[:, mc, :], ALU.mult, ALU.add, (pyr, "modt", "xT"), ("xT",))
            layer_norm(l, 0, 1)
            modulate(l, 64, 48)
            S.barrier()
            for fc in range(FC):
                pg_, pgr_ = proj(w_gate[l, fc], KC, hT_rhs, ("hT",), banks=(0, 1, 2, 3))
                pu, pur = proj(w_up[l, fc], KC, hT_rhs, ("hT",), banks=(0, 1, 2, 3))
                ta = tbuf[fc % 2]
                act(ta[:], pg_, AF.Silu, (pgr_,), (f"tbuf{fc % 2}",))
                tt(hmid[:, fc, :], pu, ta[:], ALU.mult, (pur, f"tbuf{fc % 2}"), ("hmid",))
            for mc in range(KC):
                b = mc % 4
                for half in range(2):
                    wt, wres = load_w(w_down[l, mc * 2 + half], 22 * 128)
                    for k in range(22):
                        fcx = half * 22 + k
                        S.op('pe', lambda: PE.matmul(ps[b][:, 0:G], wt[:, k * 128:(k + 1) * 128], hmid[:, fcx, :],
                                                     start=(fcx == 0), stop=(fcx == FC - 1)), (wres, "hmid"), (PS[b],))
                stt(xT[:, mc, :], ps[b][:, 0:G], modt[:, l, 80 + mc:81 + mc], xT[:, mc, :], ALU.mult, ALU.add, (PS[b], "modt", "xT"), ("xT",))
            layer_norm(l, 2, 3)
            S.dma('sp', dst[:, :, t0:t0 + G], xT[:], ("xT",), (), "xst")
    S.finish()
    return nc


def _fm_tiles(W, nkc):
    K, N = W.shape
    return np.ascontiguousarray(W.reshape(nkc, 128, N // 128, 128).transpose(2, 1, 0, 3).reshape(N // 128, 128, nkc * 128))


def _col(v):
    sh = v.shape
    n = sh[-1] // 128
    a = v.reshape(sh[:-1] + (n, 128))
    return np.ascontiguousarray(np.moveaxis(a, -1, 0))


def prep_weights(inp, DEPTH):
    f = lambda a: np.asarray(a, dtype=np.float32)
    w_in = f(inp['w_in'])
    out = {}
    out['w_ada'] = np.stack([_fm_tiles(f(inp['w_ada'][l]), KC) for l in range(DEPTH)])
    out['b_ada'] = _col(f(inp['b_ada']))
    fm, ini, inw, ing = [], [], [], []
    for l in range(DEPTH):
        W = w_in[l]
        qa, fa, ia, ga = W[:, 0:1024], W[:, 1024:2048], W[:, 2048:3072], W[:, 3072:4096]
        cqw, ckvw, kiw, iww = W[:, 4096:4608], W[:, 4608:4864], W[:, 4864:4928], W[:, 4928:4944]
        gaw, gbw = W[:, 4944:6992], W[:, 6992:9040]
        cols = np.concatenate([fa, qa, ga, cqw, ckvw, kiw, kiw], axis=1)
        fm.append(_fm_tiles(cols, KC))
        ini.append(np.stack([np.ascontiguousarray(ia[:, q * 256:(q + 1) * 256].reshape(KC, 128, 256).transpose(1, 0, 2).reshape(128, KC * 256))
                             for q in range(4)]))
        inw.append(np.ascontiguousarray(iww.reshape(KC, 128, 16).transpose(1, 0, 2).reshape(128, KC * 16)))
        ing.append(_fm_tiles(np.concatenate([gaw, gbw], axis=1), KC))
    out['w_infm'] = np.stack(fm); out['w_ini'] = np.stack(ini); out['w_inw'] = np.stack(inw); out['w_ing'] = np.stack(ing)
    out['lbl'] = _col(f(inp['lb_logits']))
    out['g_hg'] = _col(f(inp['g_hg']))
    out['g_cq'] = _col(f(inp['g_cq']))
    out['g_ckv'] = _col(f(inp['g_ckv']))
    out['w_iq'] = np.stack([_fm_tiles(f(inp['w_iq'][l]), 4) for l in range(DEPTH)])
    out['w_uq'] = np.stack([_fm_tiles(f(inp['w_uq'][l]), 4) for l in range(DEPTH)])
    wuv = f(inp['w_uv'])
    t = np.zeros((DEPTH, 8, 128, 4, 128), np.float32)
    for h in range(16):
        for cc in range(2):
            t[:, h // 2, :, (h % 2) * 2 + cc, (h % 2) * 64:(h % 2) * 64 + 64] = wuv[:, h, cc * 128:(cc + 1) * 128, :]
    out['w_uv'] = t.reshape(DEPTH, 8, 128, 512)
    out['w_pa'] = np.stack([_fm_tiles(f(inp['w_pa'][l]), 8) for l in range(DEPTH)])
    out['w_pb'] = np.stack([_fm_tiles(f(inp['w_pb'][l]), 8) for l in range(DEPTH)])
    out['w_out'] = np.stack([_fm_tiles(f(inp['w_out'][l]), KC) for l in range(DEPTH)])
    out['w_gate'] = np.stack([_fm_tiles(f(inp['w_gate'][l]), KC) for l in range(DEPTH)])
    out['w_up'] = np.stack([_fm_tiles(f(inp['w_up'][l]), KC) for l in range(DEPTH)])
    wd = []
    for l in range(DEPTH):
        t_ = _fm_tiles(f(inp['w_down'][l]), FC)
        wd.append(t_.reshape(16, 128, 2, 22 * 128).transpose(0, 2, 1, 3).reshape(32, 128, 22 * 128))
    out['w_down'] = np.ascontiguousarray(np.stack(wd))
    out['lnp'] = np.ascontiguousarray(np.stack([_col(f(inp[k])) for k in ('ln1_g', 'ln1_b', 'ln2_g', 'ln2_b')], axis=2))
    out['c_ident'] = np.eye(128, dtype=np.float32)
    out['c_tri'] = np.triu(np.ones((64, 64), np.float32))
    r = np.ones((128, G), np.float32); r[:, ::64] = 0.0
    out['c_reset'] = r
    return out


_CACHE = {}


def run(inputs, L, DEPTH, cores):
    key = (L, DEPTH)
    if key not in _CACHE:
        _CACHE[key] = build(L, DEPTH)
    nc = _CACHE[key]
    wts = prep_weights(inputs, DEPTH)
    x = np.asarray(inputs['x'], np.float32)
    c = np.asarray(inputs['c'], np.float32)
    B = x.shape[0]
    in_maps = []
    for i in range(cores):
        b = i % B
        m = dict(wts)
        m['xin'] = np.ascontiguousarray(x[b].T.reshape(KC, 128, L).transpose(1, 0, 2))
        m['cvec'] = np.ascontiguousarray(c[b].reshape(KC, 128).T)
        in_maps.append(m)
    res = run_bass_kernel_spmd(nc, in_maps, core_ids=list(range(cores)))
    out = np.empty((B, L, D), np.float32)
    for b in range(B):
        o = res.results[b]["xout"]
        out[b] = o.transpose(1, 0, 2).reshape(D, L).T
    return out


def kernel(**inputs):
    return run(inputs, 4096, 4, 4)
```

```python
import numpy as np
from contextlib import ExitStack
import concourse.bass as bass
import concourse.mybir as mybir
from concourse.bass_utils import run_bass_kernel_spmd

F32, BF16 = mybir.dt.float32, mybir.dt.bfloat16
AF = mybir.ActivationFunctionType
ALU = mybir.AluOpType
AX = mybir.AxisListType

D = 2048
KC = 16
HG = 8
DFF = 5632
FC = 44
NH = 16
G = 256
NQB = G // 128
CH = G // 64
ALPHA_ = 8.0 ** 0.25
SLOPES = [float(2.0 ** (-8.0 * (i + 1) / 16)) for i in range(16)]
BIG = 1.0e30


class Sched:
    def __init__(self, nc, es):
        self.nc, self.es = nc, es
        self.eng = {'pe': nc.tensor, 'act': nc.scalar, 'dve': nc.vector, 'pool': nc.gpsimd, 'sp': nc.sync}
        self.sem, self.cnt, self.nsem = {}, {}, 0
        for e in self.eng:
            self._newsem(e)
        self.lastw, self.readers = {}, {}
        self.waited = {e: {} for e in self.eng}
        self.dsem = {}

    def _newsem(self, e):
        s = self.es.enter_context(self.nc.semaphore(f"s{self.nsem}_{e}"))
        self.nsem += 1
        self.sem[e] = s
        self.cnt[e] = 0

    def _wait(self, e, tk, raw):
        sem, val, src = tk
        if src == e and (e in ('pe', 'sp', 'pool') or not raw):
            return
        w = self.waited[e]
        if w.get(id(sem), 0) >= val:
            return
        self.eng[e].wait_ge(sem, val)
        w[id(sem)] = val

    def _deps(self, e, reads, writes):
        for r in reads:
            t = self.lastw.get(r)
            if t:
                self._wait(e, t, True)
        for w_ in writes:
            t = self.lastw.get(w_)
            if t:
                self._wait(e, t, False)
            for t in self.readers.get(w_, {}).values():
                self._wait(e, t, False)

    def _commit(self, tk, reads, writes):
        for w_ in writes:
            self.lastw[w_] = tk
            self.readers[w_] = {}
        for r in reads:
            d = self.readers.setdefault(r, {})
            d[id(tk[0])] = tk

    def op(self, e, fn, reads=(), writes=()):
        self._deps(e, reads, writes)
        ins = fn()
        if self.cnt[e] >= 30000:
            self._newsem(e)
        self.cnt[e] += 1
        ins.then_inc(self.sem[e], 1)
        tk = (self.sem[e], self.cnt[e], e)
        self._commit(tk, reads, writes)
        return tk

    def dma(self, q, out, in_, reads, writes, res):
        if res not in self.dsem:
            self.dsem[res] = [self.es.enter_context(self.nc.semaphore(f"d{len(self.dsem)}")), 0]
        ds = self.dsem[res]
        if ds[1] >= 30000 * 16:
            ds[0] = self.es.enter_context(self.nc.semaphore(f"d{len(self.dsem)}_{self.nsem}"))
            self.nsem += 1
            ds[1] = 0
        self._deps(q, reads, writes)
        ins = self.eng[q].dma_start(out=out, in_=in_)
        ds[1] += 16
        ins.then_inc(ds[0], 16)
        tk = (ds[0], ds[1], 'dma')
        self._commit(tk, reads, writes)
        return tk

    def barrier(self, engs=('pe', 'act', 'dve')):
        for e in engs:
            for o in engs:
                if o != e and self.cnt[o] > 0:
                    self._wait(e, (self.sem[o], self.cnt[o], o), True)

    def finish(self):
        for res, (sem, cnt) in self.dsem.items():
            if cnt:
                self._wait('sp', (sem, cnt, 'dma'), True)
        for o in self.eng:
            if o != 'sp' and self.cnt[o] > 0:
                self._wait('sp', (self.sem[o], self.cnt[o], o), True)


def build(L, DEPTH):
    NG = L // G
    NSB = L // 128
    NSEL = min(256, L // 4)
    NROUND = NSEL // 8
    nc = bass.Bass("TRN2", target_bir_lowering=False)
    es = ExitStack()

    def din(name, shape):
        return nc.dram_tensor(name, list(shape), F32, kind="ExternalInput").ap()

    xin = din("xin", [128, KC, L])
    xout = nc.dram_tensor("xout", [128, KC, L], F32, kind="ExternalOutput").ap()
    xs = [nc.dram_tensor(f"xs{i}", [128, KC, L], F32, kind="Internal").ap() for i in range(2)]
    cvec = din("cvec", [128, KC])
    w_ada = din("w_ada", [DEPTH, 96, 128, KC * 128])
    b_ada = din("b_ada", [128, DEPTH, 96])
    w_infm = din("w_infm", [DEPTH, 31, 128, KC * 128])
    w_ini = din("w_ini", [DEPTH, 4, 128, KC * 256])
    w_inw = din("w_inw", [DEPTH, 128, KC * 16])
    w_ing = din("w_ing", [DEPTH, 32, 128, KC * 128])
    lbl = din("lbl", [128, DEPTH, HG])
    g_hg = din("g_hg", [128, DEPTH, HG])
    g_cq = din("g_cq", [128, DEPTH, 4])
    g_ckv = din("g_ckv", [128, DEPTH, 2])
    w_iq = din("w_iq", [DEPTH, 8, 128, 4 * 128])
    w_uq = din("w_uq", [DEPTH, 32, 128, 4 * 128])
    w_uv = din("w_uv", [DEPTH, 8, 128, 4 * 128])
    w_pa = din("w_pa", [DEPTH, 16, 128, 8 * 128])
    w_pb = din("w_pb", [DEPTH, 16, 128, 8 * 128])
    w_out = din("w_out", [DEPTH, 16, 128, KC * 128])
    w_gate = din("w_gate", [DEPTH, FC, 128, KC * 128])
    w_up = din("w_up", [DEPTH, FC, 128, KC * 128])
    w_down = din("w_down", [DEPTH, 32, 128, 22 * 128])
    lnp = din("lnp", [128, DEPTH, 4, KC])
    c_ident = din("c_ident", [128, 128])
    c_tri = din("c_tri", [64, 64])
    c_reset = din("c_reset", [128, G])

    def sb(name, shape, dt):
        return es.enter_context(nc.sbuf_tensor(name, list(shape), dt))

    S = Sched(nc, es)
    V, A, P, PE = nc.vector, nc.scalar, nc.gpsimd, nc.tensor

    wsrc = dict(w_infm=w_infm, w_ini=w_ini, w_inw=w_inw, w_ing=w_ing, w_iq=w_iq, w_uq=w_uq, w_uv=w_uv, w_pa=w_pa,
                w_pb=w_pb, w_out=w_out, w_gate=w_gate, w_up=w_up, w_down=w_down)
    wbf = {}
    for name, a in wsrc.items():
        wbf[name] = nc.dram_tensor(name + "_bf", list(a.shape), BF16, kind="Internal").ap()
    w_infm, w_ini, w_inw, w_ing, w_iq, w_uq, w_uv, w_pa, w_pb, w_out, w_gate, w_up, w_down = [
        wbf[k] for k in ("w_infm", "w_ini", "w_inw", "w_ing", "w_iq", "w_uq", "w_uv", "w_pa", "w_pb", "w_out", "w_gate", "w_up", "w_down")]
    cur_layer = [0]

    xT = sb("xT", [128, KC, G], F32)
    hT = sb("hT", [128, KC, G], BF16)
    R1 = sb("R1", [128, 2 * L * 4], mybir.dt.uint8) if False else None
    r1_bytes = max(2 * L * 4, FC * G * 2, 28 * 1024)
    R1 = sb("R1", [128, r1_bytes // 4], F32)
    R2 = sb("R2", [128, max(NSB * G * 2, 16 * 1024) // 4], F32)

    def view(reg, off_bytes, shape, dt):
        esz = 2 if dt == BF16 else 4
        n = int(np.prod(shape[1:]))
        a = reg[0:shape[0], off_bytes // 4:(off_bytes + n * esz) // 4]
        if dt == BF16:
            a = a.bitcast(BF16)
        if len(shape) == 3:
            a = a.rearrange("p (a b) -> p a b", b=shape[2])
        elif len(shape) == 4:
            a = a.rearrange("p (a b c) -> p a b c", b=shape[2], c=shape[3])
        return a

    o = 0
    ke = view(R1, o, [128, HG, G], BF16); o += HG * G * 2
    qe = view(R1, o, [128, HG, G], BF16); o += HG * G * 2
    sgT = view(R1, o, [128, HG, G], BF16); o += HG * G * 2
    vtm = view(R1, o, [64, CH, 1024], BF16); o += CH * 1024 * 2
    hgt = []
    for i in range(7):
        hgt.append(view(R1, o, [128, G], F32)); o += G * 4
    hg_f, hg_kk, hg_lf, hg_b, hg_br, hg_e1, hg_e2 = hgt
    assert o <= r1_bytes, (o, r1_bytes)
    isc = view(R1, 0, [128, L], F32)
    work = view(R1, L * 4, [128, L], F32)
    hmid = view(R1, 0, [128, FC, G], BF16)
    ndT = view(R2, 0, [128, NSB, G], BF16)
    merged = view(R2, 0, [128, KC, G], BF16)
    o = KC * G * 2
    tmpA = view(R2, o, [128, G], F32); o += G * 4
    tmpB = view(R2, o, [128, G], F32); o += G * 4
    tmpC = view(R2, o, [128, G], F32); o += G * 4

    yaT = sb("yaT", [128, HG, G], BF16)
    ybT = sb("ybT", [128, 8, G], BF16)
    Sst = sb("Sst", [128, HG, 128], F32)
    Sr = sb("Sr", [128, 2, 128], BF16)
    scm = sb("scm", [64, 2, 64], BF16)
    keT = sb("keT", [64, 2, CH * 128], BF16)
    tmpSU = sb("tmpSU", [128, 2, 128], F32)
    hg_sc = sb("hg_sc", [128, HG, 3, CH], F32)
    cq = sb("cq", [128, 4, G], F32)
    ckvr = sb("ckvr", [128, 2, G], F32)
    cqn = sb("cqn", [128, 4, G], BF16)
    sqb = [sb(f"sqb{i}", [128, G], BF16) for i in range(2)]
    vbb = [sb(f"vbb{i}", [128, G], BF16) for i in range(2)]
    st_sd = sb("st_sd", [128, G], F32)
    st_rs = sb("st_rs", [128, G], F32)
    st_mean = sb("st_mean", [128, G], F32)
    st_var = sb("st_var", [128, G], F32)
    oTs = sb("oTs", [128, G], F32)
    qidxT = sb("qidxT", [128, 8, G], BF16)
    idxw = sb("idxw", [128, NQB, NH], F32)
    diag = sb("diag", [128, NH, 128], BF16)
    Rb = [sb(f"Rb{i}", [128, 512], BF16) for i in range(4)]
    m8 = sb("m8", [128, 8], F32)
    thr = sb("thr", [128, 1], F32)
    ndmin = sb("ndmin", [128, 1], F32)
    qbT = sb("qbT", [128, 2, 2, G], BF16)
    tbuf = [sb(f"tbuf{i}", [128, G], F32) for i in range(4)]
    PTb = [sb(f"PT{i}", [128, G], BF16) for i in range(4)]
    obT = sb("obT", [128, 2, 2, G], BF16)
    rden = sb("rden", [128, G], F32)
    ckvT_all = sb("ckvT_all", [128, 2, L], BF16)
    ckv_tm = sb("ckv_tm", [128, NSB, 256], BF16)
    kidxT_all = sb("kidxT_all", [128, L], BF16)
    NW = 4
    wst = [sb(f"wst{i}", [128, 22 * 128], BF16) for i in range(NW)]
    wtm = sb("wtm", [128, KC * 256], BF16)
    wtmw = sb("wtmw", [128, KC * 16], BF16)
    ident_bf = sb("ident_bf", [128, 128], BF16)
    ident_f = sb("ident_f", [128, 128], F32)
    ones_bf = sb("ones_bf", [128, 128], BF16)
    tri = sb("tri", [64, 64], F32)
    resetm = sb("resetm", [128, G], F32)
    modt = sb("modt", [128, DEPTH, 96], F32)
    badat = sb("badat", [128, DEPTH, 96], F32)
    lnpt = sb("lnpt", [128, DEPTH, 4, KC], F32)
    lbt = sb("lbt", [128, DEPTH, HG], F32)
    omlt = sb("omlt", [128, DEPTH, HG], F32)
    lbtmp = sb("lbtmp", [128, 3, HG], F32)
    ghgt = sb("ghgt", [128, DEPTH, HG], F32)
    gcqt = sb("gcqt", [128, DEPTH, 4], F32)
    gckvt = sb("gckvt", [128, DEPTH, 2], F32)
    ct = sb("ct", [128, KC], F32)
    condb = sb("condb", [128, KC], BF16)

    ps = [es.enter_context(nc.psum_tensor(f"ps{i}", [128, 512], F32)) for i in range(8)]
    PS = [f"ps{i}" for i in range(8)]
    psT = ps[7][:].bitcast(BF16)

    cst = [view(R2, i * 4096, [128, 2048], BF16) for i in range(2)]
    ci = 0
    for l in range(DEPTH):
        for name, a in wsrc.items():
            s3 = a[l] if len(a.shape) == 4 else a[l:l + 1]
            d3 = wbf[name][l] if len(a.shape) == 4 else wbf[name][l:l + 1]
            T_, _, cols = s3.shape
            csz = cols if cols <= 2048 else cols // 2
            for t_ in range(T_):
                for c0 in range(0, cols, csz):
                    k = ci % 2
                    ci += 1
                    S.dma('pool', cst[k][:, 0:csz], s3[t_, :, c0:c0 + csz], (), (f"cst{k}",), f"cst{k}")
                    S.dma('sp', d3[t_, :, c0:c0 + csz], cst[k][:, 0:csz], (f"cst{k}",), (f"wcast{l}",), f"wcast{l}")
    wslot = [0]

    def load_w(src, ncols, cast=False):
        i = wslot[0] % NW
        wslot[0] += 1
        if cast:
            S.dma('pool', wst[i][:, 0:ncols], src, (), (f"wst{i}",), f"wst{i}")
        else:
            q = 'sp' if (wslot[0] % 2) else 'pool'
            S.dma(q, wst[i][:, 0:ncols], src, (f"wcast{cur_layer[0]}",), (f"wst{i}",), f"wst{i}")
        return wst[i], f"wst{i}"

    pctr = [0]

    def proj(src, nkc, rhs_fn, rhs_res, banks=(0, 1, 4, 5), n=G):
        wt, wres = load_w(src, nkc * 128)
        b = banks[pctr[0] % len(banks)]
        pctr[0] += 1
        for kc in range(nkc):
            S.op('pe', lambda kc=kc: PE.matmul(ps[b][:, 0:n], wt[:, kc * 128:(kc + 1) * 128], rhs_fn(kc),
                                               start=(kc == 0), stop=(kc == nkc - 1)),
                 reads=(wres,) + tuple(rhs_res), writes=(PS[b],))
        return ps[b][:, 0:n], PS[b]

    def act(out, in_, func, reads, writes, scale=1.0, bias=0.0):
        return S.op('act', lambda: A.activation(out=out, in_=in_, func=func, scale=scale, bias=bias), reads, writes)

    def ts(out, in0, s1, s2, op0, op1, reads, writes):
        if op1 is None:
            return S.op('dve', lambda: V.tensor_scalar(out=out, in0=in0, scalar1=s1, scalar2=None, op0=op0), reads, writes)
        return S.op('dve', lambda: V.tensor_scalar(out=out, in0=in0, scalar1=s1, scalar2=s2, op0=op0, op1=op1), reads, writes)

    def tt(out, in0, in1, op, reads, writes):
        return S.op('dve', lambda: V.tensor_tensor(out=out, in0=in0, in1=in1, op=op), reads, writes)

    def stt(out, in0, sc, in1, op0, op1, reads, writes):
        return S.op('dve', lambda: V.scalar_tensor_tensor(out=out, in0=in0, scalar=sc, in1=in1, op0=op0, op1=op1), reads, writes)

    def rstd_from_sum(psum_ap, psres, inv_n, eps):
        ts(st_var[:], psum_ap, inv_n, eps, ALU.mult, ALU.add, (psres,), ("st_var",))
        act(st_sd[:], st_var[:], AF.Sqrt, ("st_var",), ("st_sd",))
        S.op('dve', lambda: V.reciprocal(out=st_rs[:], in_=st_sd[:]), ("st_sd",), ("st_rs",))

    S.dma('pool', ident_bf[:], c_ident, (), ("ident_bf",), "c0")
    S.dma('sp', ident_f[:], c_ident, (), ("ident_f",), "c1")
    S.dma('sp', tri[:], c_tri, (), ("tri",), "c2")
    S.dma('sp', resetm[:], c_reset, (), ("resetm",), "c3")
    S.dma('sp', badat[:], b_ada, (), ("badat",), "c4")
    S.dma('sp', lnpt[:], lnp, (), ("lnpt",), "c5")
    S.dma('sp', lbt[:], lbl, (), ("lbt",), "c6")
    S.dma('sp', ghgt[:], g_hg, (), ("ghgt",), "c7")
    S.dma('sp', gcqt[:], g_cq, (), ("gcqt",), "c8")
    S.dma('sp', gckvt[:], g_ckv, (), ("gckvt",), "c9")
    S.dma('sp', ct[:], cvec, (), ("ct",), "c10")
    S.op('dve', lambda: V.memset(ones_bf[:], 1.0), (), ("ones_bf",))
    S.op('dve', lambda: V.memset(Sst[:], 0.0), (), tuple(f"Sst{h}" for h in range(HG)))
    act(condb[:], ct[:], AF.Silu, ("ct",), ("condb",))
    for l in range(DEPTH):
        for mc in range(96):
            wt, wres = load_w(w_ada[l, mc], KC * 128, cast=True)
            for kc in range(KC):
                S.op('pe', lambda kc=kc: PE.matmul(ps[0][:, mc:mc + 1], wt[:, kc * 128:(kc + 1) * 128],
                                                   condb[:, kc:kc + 1], start=(kc == 0), stop=(kc == KC - 1)),
                     (wres, "condb"), (PS[0],))
        tt(modt[:, l, :], ps[0][:, 0:96], badat[:, l, :], ALU.add, (PS[0], "badat"), ("modt",))
    for l in range(DEPTH):
        for c0 in (16, 32, 64, 80):
            ts(modt[:, l, c0:c0 + 16], modt[:, l, c0:c0 + 16], 1.0, None, ALU.add, None, ("modt",), ("modt",))
    mx, sm, rc = lbtmp[:, 0, :], lbtmp[:, 1, :], lbtmp[:, 2, :]
    S.op('dve', lambda: V.tensor_copy(out=mx, in_=lbt[:, 0, :]), ("lbt",), ("lbtmp",))
    for l in range(1, DEPTH):
        tt(mx, mx, lbt[:, l, :], ALU.max, ("lbt", "lbtmp"), ("lbtmp",))
    for l in range(DEPTH):
        tt(lbt[:, l, :], lbt[:, l, :], mx, ALU.subtract, ("lbt", "lbtmp"), ("lbt",))
    act(lbt[:], lbt[:], AF.Exp, ("lbt",), ("lbt",))
    S.op('dve', lambda: V.tensor_copy(out=sm, in_=lbt[:, 0, :]), ("lbt",), ("lbtmp",))
    for l in range(1, DEPTH):
        tt(sm, sm, lbt[:, l, :], ALU.add, ("lbt", "lbtmp"), ("lbtmp",))
    S.op('dve', lambda: V.reciprocal(out=rc, in_=sm), ("lbtmp",), ("lbtmp",))
    for l in range(DEPTH):
        tt(lbt[:, l, :], lbt[:, l, :], rc, ALU.mult, ("lbt", "lbtmp"), ("lbt",))
    S.op('dve', lambda: V.memset(lbt[:, 0, :], 0.0), ("lbt",), ("lbt",))
    for l in range(2, DEPTH):
        tt(lbt[:, l, :], lbt[:, l, :], lbt[:, l - 1, :], ALU.add, ("lbt",), ("lbt",))
    ts(omlt[:], lbt[:], -1.0, 1.0, ALU.mult, ALU.add, ("lbt",), ("omlt",))

    def layer_norm(l, gi, bi):
        for mc in range(KC):
            act(vbb[mc % 2][:], xT[:, mc, :], AF.Copy, ("xT",), (f"vbb{mc % 2}",))
            act(sqb[mc % 2][:], xT[:, mc, :], AF.Square, ("xT",), (f"sqb{mc % 2}",))
            S.op('pe', lambda: PE.matmul(ps[4][:, 0:G], ones_bf[:], vbb[mc % 2][:], start=(mc == 0), stop=(mc == KC - 1)),
                 ("ones_bf", f"vbb{mc % 2}"), (PS[4],))
            S.op('pe', lambda: PE.matmul(ps[5][:, 0:G], ones_bf[:], sqb[mc % 2][:], start=(mc == 0), stop=(mc == KC - 1)),
                 ("ones_bf", f"sqb{mc % 2}"), (PS[5],))
        ts(st_mean[:], ps[4][:, 0:G], 1.0 / D, None, ALU.mult, None, (PS[4],), ("st_mean",))
        tt(st_sd[:], st_mean[:], st_mean[:], ALU.mult, ("st_mean",), ("st_sd",))
        stt(st_var[:], ps[5][:, 0:G], 1.0 / D, st_sd[:], ALU.mult, ALU.subtract, (PS[5], "st_sd"), ("st_var",))
        ts(st_var[:], st_var[:], 1e-5, None, ALU.add, None, ("st_var",), ("st_var",))
        act(st_sd[:], st_var[:], AF.Sqrt, ("st_var",), ("st_sd",))
        S.op('dve', lambda: V.reciprocal(out=st_rs[:], in_=st_sd[:]), ("st_sd",), ("st_rs",))
        for mc in range(KC):
            tt(xT[:, mc, :], xT[:, mc, :], st_mean[:], ALU.subtract, ("xT", "st_mean"), ("xT",))
            tt(xT[:, mc, :], xT[:, mc, :], st_rs[:], ALU.mult, ("xT", "st_rs"), ("xT",))
            act(xT[:, mc, :], xT[:, mc, :], AF.Identity, ("xT", "lnpt"), ("xT",),
                scale=lnpt[:, l, gi, mc:mc + 1], bias=lnpt[:, l, bi, mc:mc + 1])

    def modulate(l, sc_off, sh_off):
        for kc in range(KC):
            act(hT[:, kc, :], xT[:, kc, :], AF.Identity, ("xT", "modt"), ("hT",),
                scale=modt[:, l, sc_off + kc:sc_off + kc + 1], bias=modt[:, l, sh_off + kc:sh_off + kc + 1])
        act(xT[:], xT[:], AF.Copy, ("xT",), ("xT",), scale=ALPHA_)

    hT_rhs = lambda kc: hT[:, kc, :]

    for l in range(DEPTH):
        cur_layer[0] = l
        src = xin if l == 0 else xs[(l - 1) % 2]
        dst = xout if l == DEPTH - 1 else xs[l % 2]
        if l > 0:
            sem, cnt = S.dsem["xst"]
            S._wait('sp', (sem, cnt, 'dma'), True)
            S.op('dve', lambda: V.memset(Sst[:], 0.0), (), tuple(f"Sst{h}" for h in range(HG)))
        for g in range(NG):
            t0 = g * G
            nsb = (t0 + G) // 128
            S.barrier()
            S.dma('sp', xT[:], src[:, :, t0:t0 + G], (), ("xT",), "xT")
            modulate(l, 16, 0)
            for h in range(HG):
                pf, pfr = proj(w_infm[l, h], KC, hT_rhs, ("hT",))
                act(hg_f[:], pf, AF.Sigmoid, (pfr,), ("hg_f",))
                ts(hg_f[:], hg_f[:], omlt[:, l, h:h + 1], lbt[:, l, h:h + 1], ALU.mult, ALU.add, ("hg_f", "omlt", "lbt"), ("hg_f",))
                ts(hg_kk[:], hg_f[:], -1.0, 1.0, ALU.mult, ALU.add, ("hg_f",), ("hg_kk",))
                ts(hg_f[:], hg_f[:], 1e-6, 1.0, ALU.max, ALU.min, ("hg_f",), ("hg_f",))
                act(hg_lf[:], hg_f[:], AF.Ln, ("hg_f",), ("hg_lf",))
                S.op('dve', lambda: V.tensor_tensor_scan(out=hg_b[:], data0=resetm[:], data1=hg_lf[:], initial=0.0,
                                                         op0=ALU.mult, op1=ALU.add), ("resetm", "hg_lf"), ("hg_b",))
                b3 = hg_b[:].rearrange("p (c t) -> p c t", t=64)
                br3 = hg_br[:].rearrange("p (c t) -> p c t", t=64)
                tt(br3, b3, b3[:, :, 31:32].to_broadcast([128, CH, 64]), ALU.subtract, ("hg_b",), ("hg_br",))
                act(hg_e1[:], hg_br[:], AF.Exp, ("hg_br",), ("hg_e1",))
                act(hg_e2[:], hg_br[:], AF.Exp, ("hg_br",), ("hg_e2",), scale=-1.0)
                act(hg_sc[:, h, 0, :], b3[:, :, 31], AF.Exp, ("hg_b",), ("hg_sc",))
                act(hg_sc[:, h, 1, :], b3[:, :, 63], AF.Exp, ("hg_b",), ("hg_sc",))
                act(hg_sc[:, h, 2, :], br3[:, :, 63], AF.Exp, ("hg_br",), ("hg_sc",))
                tt(ke[:, h, :], hg_kk[:], hg_e2[:], ALU.mult, ("hg_kk", "hg_e2"), ("ke",))
                pq, pqr = proj(w_infm[l, 8 + h], KC, hT_rhs, ("hT",))
                tt(qe[:, h, :], pq, hg_e1[:], ALU.mult, (pqr, "hg_e1"), ("qe",))
                pg, pgr = proj(w_infm[l, 16 + h], KC, hT_rhs, ("hT",))
                act(sgT[:, h, :], pg, AF.Silu, (pgr,), ("sgT",))
            for mc in range(4):
                p_, pr = proj(w_infm[l, 24 + mc], KC, hT_rhs, ("hT",))
                act(cq[:, mc, :], p_, AF.Copy, (pr,), ("cq",))
            for mc in range(2):
                p_, pr = proj(w_infm[l, 28 + mc], KC, hT_rhs, ("hT",))
                act(ckvr[:, mc, :], p_, AF.Copy, (pr,), ("ckvr",))
            p_, pr = proj(w_infm[l, 30], KC, hT_rhs, ("hT",))
            act(kidxT_all[:, t0:t0 + G], p_, AF.Copy, (pr,), ("kidxT_all",))
            for kc in range(4):
                act(sqb[kc % 2][:], cq[:, kc, :], AF.Square, ("cq",), (f"sqb{kc % 2}",))
                S.op('pe', lambda: PE.matmul(ps[6][:, 0:G], ones_bf[:], sqb[kc % 2][:], start=(kc == 0), stop=(kc == 3)),
                     ("ones_bf", f"sqb{kc % 2}"), (PS[6],))
            rstd_from_sum(ps[6][:, 0:G], PS[6], 1.0 / 512, 1e-6)
            for kc in range(4):
                stt(cqn[:, kc, :], cq[:, kc, :], gcqt[:, l, kc:kc + 1], st_rs[:], ALU.mult, ALU.mult, ("cq", "gcqt", "st_rs"), ("cqn",))
            for kc in range(2):
                act(sqb[kc % 2][:], ckvr[:, kc, :], AF.Square, ("ckvr",), (f"sqb{kc % 2}",))
                S.op('pe', lambda: PE.matmul(ps[6][:, 0:G], ones_bf[:], sqb[kc % 2][:], start=(kc == 0), stop=(kc == 1)),
                     ("ones_bf", f"sqb{kc % 2}"), (PS[6],))
            rstd_from_sum(ps[6][:, 0:G], PS[6], 1.0 / 256, 1e-6)
            for kc in range(2):
                stt(ckvT_all[:, kc, t0:t0 + G], ckvr[:, kc, :], gckvt[:, l, kc:kc + 1], st_rs[:], ALU.mult, ALU.mult,
                    ("ckvr", "gckvt", "st_rs"), ("ckvT_all",))
            for qb in range(NQB):
                for cc in range(2):
                    j = qb * 2 + cc
                    S.op('pe', lambda: PE.transpose(out=psT[:, j * 128:(j + 1) * 128],
                                                    in_=ckvT_all[:, cc, t0 + qb * 128:t0 + (qb + 1) * 128], identity=ident_bf[:]),
                         ("ckvT_all", "ident_bf"), (PS[7],))
            for qb in range(NQB):
                act(ckv_tm[:, t0 // 128 + qb, :], psT[:, qb * 256:(qb + 1) * 256], AF.Copy, (PS[7],), ("ckv_tm",))
            for qtr in range(4):
                S.dma('sp', wtm[:], w_ini[l, qtr], (f"wcast{l}",), ("wtm",), "wtm")
                for c in range(CH):
                    b = 2 + (c % 2)
                    for kc in range(KC):
                        S.op('pe', lambda kc=kc: PE.matmul(ps[b][0:64, 0:256], hT[:, kc, c * 64:(c + 1) * 64],
                                                           wtm[:, kc * 256:(kc + 1) * 256], start=(kc == 0), stop=(kc == KC - 1)),
                             ("hT", "wtm"), (PS[b],))
                    act(vtm[:, c, qtr * 256:(qtr + 1) * 256], ps[b][0:64, 0:256], AF.Copy, (PS[b],), ("vtm",))
            S.dma('sp', wtmw[:], w_inw[l], (f"wcast{l}",), ("wtmw",), "wtmw")
            for qb in range(NQB):
                b = 2 + (qb % 2)
                for kc in range(KC):
                    S.op('pe', lambda kc=kc: PE.matmul(ps[b][:, 0:NH], hT[:, kc, qb * 128:(qb + 1) * 128],
                                                       wtmw[:, kc * 16:(kc + 1) * 16], start=(kc == 0), stop=(kc == KC - 1)),
                         ("hT", "wtmw"), (PS[b],))
                act(idxw[:, qb, :], ps[b][:, 0:NH], AF.Copy, (PS[b],), ("idxw",), scale=1.0 / 32.0)
            for wv in range(4):
                for j in range(2):
                    h = wv * 2 + j
                    half = j * 512
                    for c in range(CH):
                        S.op('pe', lambda: PE.transpose(out=psT[0:64, half + c * 128:half + (c + 1) * 128], in_=ke[:, h, c * 64:(c + 1) * 64],
                                                        identity=ident_bf[:]), ("ke", "ident_bf"), (PS[7],))
                for j in range(2):
                    act(keT[:, j, :], psT[0:64, j * 512:j * 512 + CH * 128], AF.Copy, (PS[7],), (f"keT{j}",))
                for c in range(CH):
                    cs = slice(c * 64, (c + 1) * 64)
                    for j in range(2):
                        h = wv * 2 + j
                        ts(Sr[:, j, :], Sst[:, h, :], hg_sc[:, h, 0, c:c + 1], None, ALU.mult, None, (f"Sst{h}", "hg_sc"), (f"Sr{j}",))
                    for j in range(2):
                        h = wv * 2 + j
                        S.op('pe', lambda: PE.matmul(ps[j][0:64, 0:64], ke[:, h, cs], qe[:, h, cs], start=True, stop=True),
                             ("ke", "qe"), (PS[j],))
                    for j in range(2):
                        tt(scm[:, j, :], ps[j][0:64, 0:64], tri[:], ALU.mult, (PS[j], "tri"), (f"scm{j}",))
                    for j in range(2):
                        h = wv * 2 + j
                        po = ps[2 + j][:, c * 64:(c + 1) * 64]
                        S.op('pe', lambda: PE.matmul(po, Sr[:, j, :], qe[:, h, cs], start=True, stop=False),
                             (f"Sr{j}", "qe"), (PS[2 + j],))
                        S.op('pe', lambda: PE.matmul(po, vtm[:, c, h * 128:(h + 1) * 128], scm[:, j, :], start=False, stop=True),
                             ("vtm", f"scm{j}"), (PS[2 + j],))
                    for j in range(2):
                        h = wv * 2 + j
                        S.op('pe', lambda: PE.matmul(ps[4 + j][:, 0:128], keT[:, j, c * 128:(c + 1) * 128],
                                                     vtm[:, c, h * 128:(h + 1) * 128], start=True, stop=True),
                             (f"keT{j}", "vtm"), (PS[4 + j],))
                    for j in range(2):
                        h = wv * 2 + j
                        ts(tmpSU[:, j, :], ps[4 + j][:, 0:128], hg_sc[:, h, 2, c:c + 1], None, ALU.mult, None,
                           (PS[4 + j], "hg_sc"), (f"tmpSU{j}",))
                        stt(Sst[:, h, :], Sst[:, h, :], hg_sc[:, h, 1, c:c + 1], tmpSU[:, j, :], ALU.mult, ALU.add,
                            (f"Sst{h}", "hg_sc", f"tmpSU{j}"), (f"Sst{h}",))
                for j in range(2):
                    h = wv * 2 + j
                    pO = ps[2 + j][:, 0:G]
                    act(sqb[j][:], pO, AF.Square, (PS[2 + j],), (f"sqb{j}",))
                    act(oTs[:], pO, AF.Copy, (PS[2 + j],), ("oTs",))
                    S.op('pe', lambda: PE.matmul(ps[6][:, 0:G], ones_bf[:], sqb[j][:], start=True, stop=True), ("ones_bf", f"sqb{j}"), (PS[6],))
                    rstd_from_sum(ps[6][:, 0:G], PS[6], 1.0 / 128, 1e-6)
                    stt(oTs[:], oTs[:], ghgt[:, l, h:h + 1], st_rs[:], ALU.mult, ALU.mult, ("oTs", "ghgt", "st_rs"), ("oTs",))
                    tt(yaT[:, h, :], oTs[:], sgT[:, h, :], ALU.mult, ("oTs", "sgT"), ("yaT",))
            for mc in range(8):
                p_, pr = proj(w_iq[l, mc], 4, lambda kc: cqn[:, kc, :], ("cqn",))
                act(qidxT[:, mc, :], p_, AF.Copy, (pr,), ("qidxT",))
            S.barrier()
            for qb in range(NQB):
                Pb = t0 + qb * 128
                n_adm = Pb + 128
                for h in range(NH):
                    ts(diag[:, h, :], ident_bf[:], idxw[:, qb, h:h + 1], None, ALU.mult, None, ("ident_bf", "idxw"), ("diag",))
                nk5 = (n_adm + 511) // 512
                steps = [(k5, h) for k5 in range(nk5) for h in range(NH)]
                PFI = 3

                def SI(i):
                    k5, h = steps[i]
                    w_ = min(512, n_adm - k5 * 512)
                    b = i % 4
                    pr0 = (h % 2) * 64
                    S.op('pe', lambda: PE.matmul(ps[b][:, 0:w_], qidxT[pr0:pr0 + 64, h // 2, qb * 128:(qb + 1) * 128],
                                                 kidxT_all[pr0:pr0 + 64, k5 * 512:k5 * 512 + w_], start=True, stop=True),
                         ("qidxT", "kidxT_all"), (PS[b],))
                for i in range(min(PFI, len(steps))):
                    SI(i)
                for i, (k5, h) in enumerate(steps):
                    if i + PFI < len(steps):
                        SI(i + PFI)
                    w_ = min(512, n_adm - k5 * 512)
                    b = i % 4
                    rb = Rb[b]
                    ab = 4 + (k5 % 2)
                    act(rb[:, 0:w_], ps[b][:, 0:w_], AF.Relu, (PS[b],), (f"Rb{b}",))
                    S.op('pe', lambda: PE.matmul(ps[ab][:, 0:w_], diag[:, h, :], rb[:, 0:w_], start=(h == 0), stop=(h == NH - 1)),
                         ("diag", f"Rb{b}"), (PS[ab],))
                    if h == NH - 1:
                        act(isc[:, k5 * 512:k5 * 512 + w_], ps[ab][:, 0:w_], AF.Copy, (PS[ab],), ("isc",))
                S.op('dve', lambda: V.memset(isc[0:64, n_adm - 64:n_adm], -BIG), ("isc",), ("isc",))
                cur, curres = isc, "isc"
                for r in range(NROUND):
                    S.op('dve', lambda: V.max(out=m8[:], in_=cur[:, 0:n_adm]), (curres,), ("m8",))
                    if r < NROUND - 1:
                        S.op('dve', lambda: V.match_replace(out=work[:, 0:n_adm], in_to_replace=m8[:], in_values=cur[:, 0:n_adm],
                                                            imm_value=-3.0e38), (curres, "m8"), ("work",))
                        cur, curres = work, "work"
                ts(thr[:], m8[:, 7:8], -1.0e29, None, ALU.max, None, ("m8",), ("thr",))
                ts(work[:, 0:n_adm], isc[:, 0:n_adm], thr[:, 0:1], BIG, ALU.is_ge, ALU.mult, ("isc", "thr"), ("work",))
                S.op('pool', lambda: P.iota(isc[:, 0:n_adm], pattern=[[1, n_adm]], base=-Pb, channel_multiplier=-1,
                                            allow_small_or_imprecise_dtypes=True), (), ("isc",))
                ts(tbuf[0][:, 0:128], isc[:, Pb:Pb + 128], -1.0, None, ALU.mult, None, ("isc",), ("tbuf0",))
                tt(isc[:, Pb:Pb + 128], isc[:, Pb:Pb + 128], tbuf[0][:, 0:128], ALU.min, ("isc", "tbuf0"), ("isc",))
                stt(work[:, 0:n_adm], work[:, 0:n_adm], -BIG, isc[:, 0:n_adm], ALU.add, ALU.add, ("work", "isc"), ("work",))
                S.op('dve', lambda: V.tensor_reduce(out=ndmin[:], in_=work[:, 0:n_adm], axis=AX.X, op=ALU.max), ("work",), ("ndmin",))
                ts(work[:, 0:n_adm], work[:, 0:n_adm], ndmin[:, 0:1], None, ALU.subtract, None, ("work", "ndmin"), ("work",))
                nb = n_adm // 128
                for s0 in range(0, nb, 4):
                    k = min(4, nb - s0)
                    bk = 5 + ((s0 // 4) % 2)
                    for j in range(k):
                        S.op('pe', lambda: PE.transpose(out=ps[bk][:, j * 128:(j + 1) * 128], in_=work[:, (s0 + j) * 128:(s0 + j + 1) * 128],
                                                        identity=ident_f[:]), ("work", "ident_f"), (PS[bk],))
                    act(ndT[:, s0:s0 + k, qb * 128:(qb + 1) * 128], ps[bk][:, 0:k * 128].rearrange("p (a b) -> p a b", b=128),
                        AF.Copy, (PS[bk],), ("ndT",))
                for s_ in range(nb, nsb):
                    S.op('dve', lambda: V.memset(ndT[:, s_, qb * 128:(qb + 1) * 128], -BIG), (), ("ndT",))
            def qb_proj(h_):
                for cc in range(2):
                    p_, pr = proj(w_uq[l, h_ * 2 + cc], 4, lambda kc: cqn[:, kc, :], ("cqn",), banks=(7,))
                    act(qbT[:, h_ % 2, cc, :], p_, AF.Copy, (pr,), (f"qbT{h_ % 2}",), scale=1.0 / 16.0)
            PF = 3
            qb_proj(0)
            for h in range(NH):
                hp = h % 2
                if h + 1 < NH:
                    qb_proj(h + 1)

                def ST(s_):
                    b = s_ % 4
                    for cc in range(2):
                        S.op('pe', lambda: PE.matmul(ps[b][:, 0:G], ckvT_all[:, cc, s_ * 128:(s_ + 1) * 128], qbT[:, hp, cc, :],
                                                     start=(cc == 0), stop=(cc == 1)), ("ckvT_all", f"qbT{hp}"), (PS[b],))
                for s_ in range(min(PF, nsb)):
                    ST(s_)
                for s_ in range(nsb):
                    if s_ + PF < nsb:
                        ST(s_ + PF)
                    b = s_ % 4
                    tb, pt = tbuf[b], PTb[b]
                    stt(tb[:], ndT[:, s_, :], SLOPES[h], ps[b][:, 0:G], ALU.mult, ALU.add, ("ndT", PS[b]), (f"tbuf{b}",))
                    act(pt[:], tb[:], AF.Exp, (f"tbuf{b}",), (f"PT{b}",))
                    S.op('pe', lambda: PE.matmul(ps[4][:, 0:G], ckv_tm[:, s_, 0:128], pt[:], start=(s_ == 0), stop=(s_ == nsb - 1)),
                         ("ckv_tm", f"PT{b}"), (PS[4],))
                    S.op('pe', lambda: PE.matmul(ps[5][:, 0:G], ckv_tm[:, s_, 128:256], pt[:], start=(s_ == 0), stop=(s_ == nsb - 1)),
                         ("ckv_tm", f"PT{b}"), (PS[5],))
                    S.op('pe', lambda: PE.matmul(ps[6][:, 0:G], ones_bf[:], pt[:], start=(s_ == 0), stop=(s_ == nsb - 1)),
                         ("ones_bf", f"PT{b}"), (PS[6],))
                act(obT[:, hp, 0, :], ps[4][:, 0:G], AF.Copy, (PS[4],), ("obT",))
                act(obT[:, hp, 1, :], ps[5][:, 0:G], AF.Copy, (PS[5],), ("obT",))
                S.op('dve', lambda: V.reciprocal(out=rden[hp * 64:hp * 64 + 64, :], in_=ps[6][hp * 64:hp * 64 + 64, 0:G]), (PS[6],), ("rden",))
                if hp == 1:
                    wt, wres = load_w(w_uv[l, h // 2], 4 * 128)
                    for i4 in range(4):
                        S.op('pe', lambda: PE.matmul(ps[7][:, 0:G], wt[:, i4 * 128:(i4 + 1) * 128], obT[:, i4 // 2, i4 % 2, :],
                                                     start=(i4 == 0), stop=(i4 == 3)), (wres, "obT"), (PS[7],))
                    tt(ybT[:, h // 2, :], ps[7][:, 0:G], rden[:], ALU.mult, (PS[7], "rden"), ("ybT",))
            S.barrier()
            for mc in range(KC):
                pa, par = proj(w_pa[l, mc], 8, lambda kc: yaT[:, kc, :], ("yaT",), banks=(0, 1, 2, 3))
                pga, pgar = proj(w_ing[l, mc], KC, hT_rhs, ("hT",), banks=(0, 1, 2, 3))
                act(tmpA[:], pga, AF.Sigmoid, (pgar,), ("tmpA",))
                tt(tmpB[:], pa, tmpA[:], ALU.mult, (par, "tmpA"), ("tmpB",))
                pb_, pbr = proj(w_pb[l, mc], 8, lambda kc: ybT[:, kc, :], ("ybT",), banks=(0, 1, 2, 3))
                pgb, pgbr = proj(w_ing[l, 16 + mc], KC, hT_rhs, ("hT",), banks=(0, 1, 2, 3))
                act(tmpC[:], pgb, AF.Sigmoid, (pgbr,), ("tmpC",))
                tt(tmpC[:], pb_, tmpC[:], ALU.mult, (pbr, "tmpC"), ("tmpC",))
                tt(merged[:, mc, :], tmpB[:], tmpC[:], ALU.add, ("tmpB", "tmpC"), ("merged",))
            for mc in range(KC):
                py, pyr = proj(w_out[l, mc], KC, lambda kc: merged[:, kc, :], ("merged",), banks=(0, 1, 2, 3))
                stt(xT[:, mc, :], py, modt[:, l, 32 + mc:33 + mc], xT[:, mc, :], ALU.mult, ALU.add, (pyr, "modt", "xT"), ("xT",))
            layer_norm(l, 0, 1)
            modulate(l, 64, 48)
            S.barrier()
            for fc in range(FC):
                pg_, pgr_ = proj(w_gate[l, fc], KC, hT_rhs, ("hT",), banks=(0, 1, 2, 3))
                pu, pur = proj(w_up[l, fc], KC, hT_rhs, ("hT",), banks=(0, 1, 2, 3))
                ta = tbuf[fc % 2]
                act(ta[:], pg_, AF.Silu, (pgr_,), (f"tbuf{fc % 2}",))
                tt(hmid[:, fc, :], pu, ta[:], ALU.mult, (pur, f"tbuf{fc % 2}"), ("hmid",))
            for mc in range(KC):
                b = mc % 4
                for half in range(2):
                    wt, wres = load_w(w_down[l, mc * 2 + half], 22 * 128)
                    for k in range(22):
                        fcx = half * 22 + k
                        S.op('pe', lambda: PE.matmul(ps[b][:, 0:G], wt[:, k * 128:(k + 1) * 128], hmid[:, fcx, :],
                                                     start=(fcx == 0), stop=(fcx == FC - 1)), (wres, "hmid"), (PS[b],))
                stt(xT[:, mc, :], ps[b][:, 0:G], modt[:, l, 80 + mc:81 + mc], xT[:, mc, :], ALU.mult, ALU.add, (PS[b], "modt", "xT"), ("xT",))
            layer_norm(l, 2, 3)
            S.dma('sp', dst[:, :, t0:t0 + G], xT[:], ("xT",), (), "xst")
    S.finish()
    return nc


def _fm_tiles(W, nkc):
    K, N = W.shape
    return np.ascontiguousarray(W.reshape(nkc, 128, N // 128, 128).transpose(2, 1, 0, 3).reshape(N // 128, 128, nkc * 128))


def _col(v):
    sh = v.shape
    n = sh[-1] // 128
    a = v.reshape(sh[:-1] + (n, 128))
    return np.ascontiguousarray(np.moveaxis(a, -1, 0))


def prep_weights(inp, DEPTH):
    f = lambda a: np.asarray(a, dtype=np.float32)
    w_in = f(inp['w_in'])
    out = {}
    out['w_ada'] = np.stack([_fm_tiles(f(inp['w_ada'][l]), KC) for l in range(DEPTH)])
    out['b_ada'] = _col(f(inp['b_ada']))
    fm, ini, inw, ing = [], [], [], []
    for l in range(DEPTH):
        W = w_in[l]
        qa, fa, ia, ga = W[:, 0:1024], W[:, 1024:2048], W[:, 2048:3072], W[:, 3072:4096]
        cqw, ckvw, kiw, iww = W[:, 4096:4608], W[:, 4608:4864], W[:, 4864:4928], W[:, 4928:4944]
        gaw, gbw = W[:, 4944:6992], W[:, 6992:9040]
        cols = np.concatenate([fa, qa, ga, cqw, ckvw, kiw, kiw], axis=1)
        fm.append(_fm_tiles(cols, KC))
        ini.append(np.stack([np.ascontiguousarray(ia[:, q * 256:(q + 1) * 256].reshape(KC, 128, 256).transpose(1, 0, 2).reshape(128, KC * 256))
                             for q in range(4)]))
        inw.append(np.ascontiguousarray(iww.reshape(KC, 128, 16).transpose(1, 0, 2).reshape(128, KC * 16)))
        ing.append(_fm_tiles(np.concatenate([gaw, gbw], axis=1), KC))
    out['w_infm'] = np.stack(fm); out['w_ini'] = np.stack(ini); out['w_inw'] = np.stack(inw); out['w_ing'] = np.stack(ing)
    out['lbl'] = _col(f(inp['lb_logits']))
    out['g_hg'] = _col(f(inp['g_hg']))
    out['g_cq'] = _col(f(inp['g_cq']))
    out['g_ckv'] = _col(f(inp['g_ckv']))
    out['w_iq'] = np.stack([_fm_tiles(f(inp['w_iq'][l]), 4) for l in range(DEPTH)])
    out['w_uq'] = np.stack([_fm_tiles(f(inp['w_uq'][l]), 4) for l in range(DEPTH)])
    wuv = f(inp['w_uv'])
    t = np.zeros((DEPTH, 8, 128, 4, 128), np.float32)
    for h in range(16):
        for cc in range(2):
            t[:, h // 2, :, (h % 2) * 2 + cc, (h % 2) * 64:(h % 2) * 64 + 64] = wuv[:, h, cc * 128:(cc + 1) * 128, :]
    out['w_uv'] = t.reshape(DEPTH, 8, 128, 512)
    out['w_pa'] = np.stack([_fm_tiles(f(inp['w_pa'][l]), 8) for l in range(DEPTH)])
    out['w_pb'] = np.stack([_fm_tiles(f(inp['w_pb'][l]), 8) for l in range(DEPTH)])
    out['w_out'] = np.stack([_fm_tiles(f(inp['w_out'][l]), KC) for l in range(DEPTH)])
    out['w_gate'] = np.stack([_fm_tiles(f(inp['w_gate'][l]), KC) for l in range(DEPTH)])
    out['w_up'] = np.stack([_fm_tiles(f(inp['w_up'][l]), KC) for l in range(DEPTH)])
    wd = []
    for l in range(DEPTH):
        t_ = _fm_tiles(f(inp['w_down'][l]), FC)
        wd.append(t_.reshape(16, 128, 2, 22 * 128).transpose(0, 2, 1, 3).reshape(32, 128, 22 * 128))
    out['w_down'] = np.ascontiguousarray(np.stack(wd))
    out['lnp'] = np.ascontiguousarray(np.stack([_col(f(inp[k])) for k in ('ln1_g', 'ln1_b', 'ln2_g', 'ln2_b')], axis=2))
    out['c_ident'] = np.eye(128, dtype=np.float32)
    out['c_tri'] = np.triu(np.ones((64, 64), np.float32))
    r = np.ones((128, G), np.float32); r[:, ::64] = 0.0
    out['c_reset'] = r
    return out


_CACHE = {}


def run(inputs, L, DEPTH, cores):
    key = (L, DEPTH)
    if key not in _CACHE:
        _CACHE[key] = build(L, DEPTH)
    nc = _CACHE[key]
    wts = prep_weights(inputs, DEPTH)
    x = np.asarray(inputs['x'], np.float32)
    c = np.asarray(inputs['c'], np.float32)
    B = x.shape[0]
    in_maps = []
    for i in range(cores):
        b = i % B
        m = dict(wts)
        m['xin'] = np.ascontiguousarray(x[b].T.reshape(KC, 128, L).transpose(1, 0, 2))
        m['cvec'] = np.ascontiguousarray(c[b].reshape(KC, 128).T)
        in_maps.append(m)
    res = run_bass_kernel_spmd(nc, in_maps, core_ids=list(range(cores)))
    out = np.empty((B, L, D), np.float32)
    for b in range(B):
        o = res.results[b]["xout"]
        out[b] = o.transpose(1, 0, 2).reshape(D, L).T
    return out


def kernel(**inputs):
    return run(inputs, 4096, 4, 4)
```
